# Optimizing a Trainium2 kernel written in Bass

```python
import jax, jax.numpy as jnp
from jax import lax
import numpy as np

D_MODEL = 1024
BATCH = 2
SEQ = 8192
DEPTH = 2

N_MIXERS = 2
BLOCK_Q = 128
EPS = 1e-6
FOX_HEADS = 16
FOX_HEAD_DIM = D_MODEL // FOX_HEADS
FOX_IN = 3 * D_MODEL + FOX_HEADS
FORGET_BIAS_INIT = 3.0
MLA_HEADS = 16
MLA_Q_RANK = 384
MLA_KV_RANK = 256
MLA_NOPE_DIM = 64
MLA_ROPE_DIM = 32
MLA_V_DIM = 64
MLA_QK_DIM = MLA_NOPE_DIM + MLA_ROPE_DIM
MLA_IN = MLA_Q_RANK + MLA_KV_RANK + MLA_ROPE_DIM
ROPE_THETA = 10000.0
N_GROUPS = 4
EXPERTS_PER_GROUP = 4
N_EXPERTS = N_GROUPS * EXPERTS_PER_GROUP
TOP_K_IN_GROUP = 2
D_EXPERT = 256

N_FOX_LAYERS = (DEPTH + 1) // 2
N_MLA_LAYERS = DEPTH // 2

kernel_name = "fox_mla_hier_moe_adaln_trunk"


def rmsnorm(x, g):
    xf = x.astype(jnp.float32)
    y = xf * lax.rsqrt(jnp.mean(xf * xf, axis=-1, keepdims=True) + EPS)
    return (y * g.astype(jnp.float32)).astype(x.dtype)


def rope(x, positions):
    half = MLA_ROPE_DIM // 2
    inv_freq = ROPE_THETA ** (-jnp.arange(0, half, dtype=jnp.float32) / half)
    ang = positions.astype(jnp.float32)[..., None] * inv_freq
    cos = jnp.cos(ang)[:, :, None, :]
    sin = jnp.sin(ang)[:, :, None, :]
    xf = x.astype(jnp.float32)
    x1, x2 = xf[..., :half], xf[..., half:]
    out = jnp.concatenate([x1 * cos - x2 * sin, x1 * sin + x2 * cos], axis=-1)
    return out.astype(x.dtype)


def causal_block_attention(q, k, v, scale, cum=None):
    B, H, S, dk = q.shape
    nb = S // BLOCK_Q
    qb = q.reshape(B, H, nb, BLOCK_Q, dk).transpose(2, 0, 1, 3, 4)
    kpos = jnp.arange(S)

    def one_block(args):
        i, qi = args
        s = jnp.einsum('bhqd,bhkd->bhqk', qi, k).astype(jnp.float32) * scale
        if cum is not None:
            ci = lax.dynamic_slice_in_dim(cum, i * BLOCK_Q, BLOCK_Q, axis=2)
            s = s + ci[..., :, None] - cum[..., None, :]
        qpos = i * BLOCK_Q + jnp.arange(BLOCK_Q)
        mask = kpos[None, :] <= qpos[:, None]
        s = jnp.where(mask, s, -jnp.inf)
        p = jax.nn.softmax(s, axis=-1)
        return jnp.einsum('bhqk,bhkd->bhqd', p.astype(v.dtype), v)

    o = lax.map(one_block, (jnp.arange(nb), qb))
    return o.transpose(1, 2, 0, 3, 4).reshape(B, H, S, v.shape[-1])


def fox_mixer(h, w_in, b_f, q_norm, k_norm, w_out):
    B, S, D = h.shape
    proj = h @ w_in
    q = proj[..., :D].reshape(B, S, FOX_HEADS, FOX_HEAD_DIM)
    k = proj[..., D:2 * D].reshape(B, S, FOX_HEADS, FOX_HEAD_DIM)
    v = proj[..., 2 * D:3 * D].reshape(B, S, FOX_HEADS, FOX_HEAD_DIM)
    f_logit = proj[..., 3 * D:]
    q = rmsnorm(q, q_norm)
    k = rmsnorm(k, k_norm)
    log_f = jax.nn.log_sigmoid((f_logit + b_f).astype(jnp.float32))
    cum = jnp.cumsum(log_f, axis=1).transpose(0, 2, 1)
    o = causal_block_attention(q.transpose(0, 2, 1, 3), k.transpose(0, 2, 1, 3),
                               v.transpose(0, 2, 1, 3), FOX_HEAD_DIM ** -0.5, cum)
    o = o.transpose(0, 2, 1, 3).reshape(B, S, D)
    return o @ w_out


def mla_mixer(h, positions, w_in, q_lat_norm, kv_lat_norm, w_uq, w_ukv, q_norm, k_norm, w_out):
    B, S, D = h.shape
    proj = h @ w_in
    cq = rmsnorm(proj[..., :MLA_Q_RANK], q_lat_norm)
    ckv = rmsnorm(proj[..., MLA_Q_RANK:MLA_Q_RANK + MLA_KV_RANK], kv_lat_norm)
    k_rope = proj[..., MLA_Q_RANK + MLA_KV_RANK:]
    q = (cq @ w_uq).reshape(B, S, MLA_HEADS, MLA_QK_DIM)
    kv = (ckv @ w_ukv).reshape(B, S, MLA_HEADS, MLA_NOPE_DIM + MLA_V_DIM)
    k_nope, v = kv[..., :MLA_NOPE_DIM], kv[..., MLA_NOPE_DIM:]
    k = jnp.concatenate(
        [k_nope, jnp.broadcast_to(k_rope[:, :, None, :], (B, S, MLA_HEADS, MLA_ROPE_DIM))], axis=-1)
    q = rmsnorm(q, q_norm)
    k = rmsnorm(k, k_norm)
    q = jnp.concatenate([q[..., :MLA_NOPE_DIM], rope(q[..., MLA_NOPE_DIM:], positions)], axis=-1)
    k = jnp.concatenate([k[..., :MLA_NOPE_DIM], rope(k[..., MLA_NOPE_DIM:], positions)], axis=-1)
    o = causal_block_attention(q.transpose(0, 2, 1, 3), k.transpose(0, 2, 1, 3),
                               v.transpose(0, 2, 1, 3), MLA_QK_DIM ** -0.5)
    o = o.transpose(0, 2, 1, 3).reshape(B, S, MLA_HEADS * MLA_V_DIM)
    return o @ w_out


def hier_moe(h, w_grp, b_grp, w_rt, b_rt, w_gate, w_up, w_down):
    B, S, D = h.shape
    T = B * S
    t = h.reshape(T, D)
    g_logits = (t @ w_grp).astype(jnp.float32) + b_grp.astype(jnp.float32)
    g_prob = jax.nn.softmax(g_logits, axis=-1)
    g_top, g_idx = lax.top_k(g_prob, 1)
    e_logits = ((t @ w_rt).astype(jnp.float32) + b_rt.astype(jnp.float32)).reshape(
        T, N_GROUPS, EXPERTS_PER_GROUP)
    sel = jnp.broadcast_to(g_idx[:, :, None], (T, 1, EXPERTS_PER_GROUP))
    in_group = jnp.take_along_axis(e_logits, sel, axis=1)[:, 0]
    e_top, e_idx = lax.top_k(in_group, TOP_K_IN_GROUP)
    w = jax.nn.softmax(e_top, axis=-1) * g_top
    expert_id = g_idx * EXPERTS_PER_GROUP + e_idx
    combine = jnp.sum(jax.nn.one_hot(expert_id, N_EXPERTS, dtype=jnp.float32) * w[..., None], axis=1)
    a = jnp.einsum('td,edf->tef', t, w_gate)
    u = jnp.einsum('td,edf->tef', t, w_up)
    hidden = jax.nn.silu(a) * u * combine[..., None].astype(t.dtype)
    out = jnp.einsum('tef,efd->td', hidden, w_down)
    return out.reshape(B, S, D)


def setup_inputs(seed: int = 0) -> dict:
    key = jax.random.key(seed)
    ks = jax.random.split(key, 32)
    D = D_MODEL
    nrm = lambda k, shape, scale: jax.random.normal(k, shape, jnp.float32) * scale
    gain = lambda k, shape: 1.0 + 0.02 * jax.random.normal(k, shape, jnp.float32)
    offsets = jax.random.randint(ks[2], (BATCH, 1), 0, 1024, dtype=jnp.int32)
    return {
        "x": nrm(ks[0], (BATCH, SEQ, D), 1.0),
        "c": nrm(ks[1], (BATCH, D), 1.0),
        "positions": offsets + jnp.arange(SEQ, dtype=jnp.int32)[None, :],
        "w_ada": nrm(ks[3], (DEPTH, D, 6 * D), 0.5 * D ** -0.5),
        "b_ada": nrm(ks[4], (DEPTH, 6 * D), 0.01),
        "mix_norm": gain(ks[5], (DEPTH, D)),
        "ffn_norm": gain(ks[6], (DEPTH, D)),
        "fox_w_in": nrm(ks[7], (N_FOX_LAYERS, D, FOX_IN), D ** -0.5),
        "fox_b_f": FORGET_BIAS_INIT + nrm(ks[8], (N_FOX_LAYERS, FOX_HEADS), 0.5),
        "fox_q_norm": gain(ks[9], (N_FOX_LAYERS, FOX_HEAD_DIM)),
        "fox_k_norm": gain(ks[10], (N_FOX_LAYERS, FOX_HEAD_DIM)),
        "fox_w_out": nrm(ks[11], (N_FOX_LAYERS, D, D), D ** -0.5),
        "mla_w_in": nrm(ks[12], (N_MLA_LAYERS, D, MLA_IN), D ** -0.5),
        "mla_q_lat_norm": gain(ks[13], (N_MLA_LAYERS, MLA_Q_RANK)),
        "mla_kv_lat_norm": gain(ks[14], (N_MLA_LAYERS, MLA_KV_RANK)),
        "mla_w_uq": nrm(ks[15], (N_MLA_LAYERS, MLA_Q_RANK, MLA_HEADS * MLA_QK_DIM), MLA_Q_RANK ** -0.5),
        "mla_w_ukv": nrm(ks[16], (N_MLA_LAYERS, MLA_KV_RANK, MLA_HEADS * (MLA_NOPE_DIM + MLA_V_DIM)),
                         MLA_KV_RANK ** -0.5),
        "mla_q_norm": gain(ks[17], (N_MLA_LAYERS, MLA_QK_DIM)),
        "mla_k_norm": gain(ks[18], (N_MLA_LAYERS, MLA_QK_DIM)),
        "mla_w_out": nrm(ks[19], (N_MLA_LAYERS, MLA_HEADS * MLA_V_DIM, D), (MLA_HEADS * MLA_V_DIM) ** -0.5),
        "moe_w_grp": nrm(ks[20], (DEPTH, D, N_GROUPS), D ** -0.5),
        "moe_b_grp": nrm(ks[21], (DEPTH, N_GROUPS), 0.01),
        "moe_w_rt": nrm(ks[22], (DEPTH, D, N_EXPERTS), D ** -0.5),
        "moe_b_rt": nrm(ks[23], (DEPTH, N_EXPERTS), 0.01),
        "moe_w_gate": nrm(ks[24], (DEPTH, N_EXPERTS, D, D_EXPERT), D ** -0.5),
        "moe_w_up": nrm(ks[25], (DEPTH, N_EXPERTS, D, D_EXPERT), D ** -0.5),
        "moe_w_down": nrm(ks[26], (DEPTH, N_EXPERTS, D_EXPERT, D), D_EXPERT ** -0.5),
    }


def reference(x, c, positions, w_ada, b_ada, mix_norm, ffn_norm,
              fox_w_in, fox_b_f, fox_q_norm, fox_k_norm, fox_w_out,
              mla_w_in, mla_q_lat_norm, mla_kv_lat_norm, mla_w_uq, mla_w_ukv, mla_q_norm, mla_k_norm,
              mla_w_out,
              moe_w_grp, moe_b_grp, moe_w_rt, moe_b_rt, moe_w_gate, moe_w_up, moe_w_down):
    c_act = jax.nn.silu(c)
    for i in range(DEPTH):
        mod = c_act @ w_ada[i] + b_ada[i]
        sh1, sc1, g1, sh2, sc2, g2 = [m[:, None, :] for m in jnp.split(mod, 6, axis=-1)]
        h = rmsnorm(x, mix_norm[i]) * (1.0 + sc1) + sh1
        j = i // N_MIXERS
        if i % N_MIXERS == 0:
            y = fox_mixer(h, fox_w_in[j], fox_b_f[j], fox_q_norm[j], fox_k_norm[j], fox_w_out[j])
        else:
            y = mla_mixer(h, positions, mla_w_in[j], mla_q_lat_norm[j], mla_kv_lat_norm[j],
                          mla_w_uq[j], mla_w_ukv[j], mla_q_norm[j], mla_k_norm[j], mla_w_out[j])
        x = x + g1 * y
        h = rmsnorm(x, ffn_norm[i]) * (1.0 + sc2) + sh2
        x = x + g2 * hier_moe(h, moe_w_grp[i], moe_b_grp[i], moe_w_rt[i], moe_b_rt[i],
                              moe_w_gate[i], moe_w_up[i], moe_w_down[i])
    return x
```

```python
import contextlib
import numpy as np
import ml_dtypes
import concourse.bass as bass
import concourse.mybir as mybir
from concourse.bass_utils import run_bass_kernel_spmd

F32 = mybir.dt.float32
BF16 = mybir.dt.bfloat16
I32 = mybir.dt.int32
ACT = mybir.ActivationFunctionType
ALU = mybir.AluOpType
AX = mybir.AxisListType

import os
ENGS = ("pe", "act", "dve", "pool", "sp")
STOP = os.environ.get("MK_STOP", "")


class StopBuild(Exception):
    pass


def stop_point(name, P):
    if STOP == name:
        P.barrier()
        P.stopped = True
        print("build stopped at", name)
    return getattr(P, "stopped", False)
D = 1024
H = 16
EPS = 1e-6
NEG = -30000.0


class Res:
    def __init__(self, name, psum=False):
        self.name = name
        self.psum = psum
        self.writers = {}
        self.readers = {}
        self.pend_w = None
        self.pend_r = set()


RING = {"sp": 24, "pool": 16, "act": 4}


class Prog:
    def __init__(self, nc, stack):
        self.nc = nc
        self.stack = stack
        self.ops = {e: [] for e in ENGS}
        self.sems = {}
        self.cnt = {e: 0 for e in ENGS}
        self.waited = {e: {} for e in ENGS}
        self.dmaval = {}
        self.pend = {e: ([], []) for e in ENGS}
        self.ninstr = {e: 0 for e in ENGS}
        self.ring_i = {e: 0 for e in RING}
        self.cc_i = 0
        for e in ENGS:
            self.sem("E_" + e)
        for e, n in RING.items():
            for i in range(n):
                self.sem(f"D_{e}{i}")
        for i in range(16):
            self.sem(f"C_{i}")

    def sem(self, name):
        if name not in self.sems:
            self.sems[name] = self.stack.enter_context(self.nc.semaphore(name))
            self.dmaval[name] = 0
        return name

    def _need(self, eng, reads, writes, extra, merge=False):
        need = {}

        def add(s, v):
            if need.get(s, 0) < v:
                need[s] = v

        own = "E_" + eng
        for r in reads:
            assert r.pend_w is None or r.pend_w == eng, f"read of {r.name}: unsignaled write by {r.pend_w}"
            for s, v in r.writers.items():
                add(s, v)
            if r.psum:
                for s, v in r.readers.items():
                    if s != own:
                        add(s, v)
        if not merge:
            for w in writes:
                assert w.pend_w is None or w.pend_w == eng, f"write of {w.name}: unsignaled write by {w.pend_w}"
                assert not (w.pend_r - {eng}), f"write of {w.name}: unsignaled reads by {w.pend_r}"
                for s, v in w.writers.items():
                    add(s, v)
                for s, v in w.readers.items():
                    add(s, v)
        for (s, v) in extra:
            add(s, v)
        if eng == "pe":
            need.pop(own, None)
        out = []
        for s, v in need.items():
            if self.waited[eng].get(s, 0) < v:
                self.waited[eng][s] = v
                out.append((s, v))
        return out

    def _emit_waits(self, eng, waits):
        for s, v in waits:
            h = self.sems[s]
            self.ops[eng].append(lambda e, h=h, v=v: e.wait_ge(h, v))

    def op(self, eng, fn, reads=(), writes=(), signal=True, extra=()):
        waits = self._need(eng, reads, writes, extra)
        self._emit_waits(eng, waits)
        self.ninstr[eng] += 1
        pr, pw = self.pend[eng]
        if not signal:
            self.ops[eng].append(lambda e, fn=fn: fn(e))
            for r in reads:
                pr.append(r)
                r.pend_r.add(eng)
            for w in writes:
                pw.append(w)
                w.pend_w = eng
            return None
        own = "E_" + eng
        self.cnt[eng] += 1
        tok = (own, self.cnt[eng])
        h = self.sems[own]
        self.ops[eng].append(lambda e, fn=fn, h=h: fn(e).then_inc(h, 1))
        for r in list(reads) + pr:
            r.readers[own] = tok[1]
            r.pend_r.discard(eng)
        for w in list(writes) + pw:
            w.writers = {own: tok[1]}
            w.readers = {}
            w.pend_w = None
        self.pend[eng] = ([], [])
        return tok

    def dma(self, eng, out, in_, sem=None, reads=(), writes=(), extra=(), merge=False, **kw):
        i = self.ring_i[eng]
        self.ring_i[eng] = (i + 1) % RING[eng]
        sem = f"D_{eng}{i}"
        extra = list(extra)
        if self.dmaval[sem] > 0:
            extra.append((sem, self.dmaval[sem]))
        waits = self._need(eng, reads, writes, extra, merge=merge)
        self._emit_waits(eng, waits)
        self.dmaval[sem] += 16
        tok = (sem, self.dmaval[sem])
        h = self.sems[sem]
        self.ninstr[eng] += 1
        self.ops[eng].append(
            lambda e, out=out, in_=in_, h=h, kw=kw: e.dma_start(out=out, in_=in_, **kw).then_inc(h, 16))
        for r in reads:
            r.readers[sem] = tok[1]
        for w in writes:
            if merge:
                w.writers[sem] = tok[1]
            else:
                w.writers = {sem: tok[1]}
                w.readers = {}
        return tok

    def collective(self, groups, in_ap, out_ap, sem=None, reads=(), writes=()):
        eng = "pool"
        sem = f"C_{self.cc_i % 16}"
        self.cc_i += 1
        extra = [(sem, self.dmaval[sem])] if self.dmaval[sem] > 0 else []
        waits = self._need(eng, reads, writes, extra)
        self._emit_waits(eng, waits)
        self.dmaval[sem] += 1
        tok = (sem, self.dmaval[sem])
        h = self.sems[sem]
        self.ninstr[eng] += 1
        self.ops[eng].append(
            lambda e: e.collective_compute("AllGather", ALU.bypass, replica_groups=groups,
                                           ins=[in_ap], outs=[out_ap]).then_inc(h))
        for r in reads:
            r.readers[sem] = tok[1]
        for w in writes:
            w.writers = {sem: tok[1]}
            w.readers = {}
        return tok

    def wait_all(self, eng, skip_cc=False):
        waits = []
        for s in self.sems:
            if skip_cc and s.startswith("C_"):
                continue
            v = self.cnt[s[2:]] if s.startswith("E_") else self.dmaval[s]
            if s == "E_" + eng:
                continue
            if v > 0 and self.waited[eng].get(s, 0) < v:
                self.waited[eng][s] = v
                waits.append((s, v))
        self._emit_waits(eng, waits)

    def barrier(self, skip_cc=False):
        for e in ENGS:
            assert not self.pend[e][0] and not self.pend[e][1], f"pending unsignaled ops on {e}"
        for e in ENGS:
            self.wait_all(e, skip_cc=skip_cc)

    def finish(self):
        self.barrier()
        nc = self.nc
        ops = self.ops
        with nc.Block() as block:
            @block.tensor
            def _(e):
                for f in ops["pe"]:
                    f(e)

            @block.scalar
            def _(e):
                for f in ops["act"]:
                    f(e)

            @block.vector
            def _(e):
                for f in ops["dve"]:
                    f(e)

            @block.gpsimd
            def _(e):
                for f in ops["pool"]:
                    f(e)

            @block.sync
            def _(e):
                for f in ops["sp"]:
                    f(e)


class Cfg:
    def __init__(self, B=2, S=8192, depth=2, G=4, EPG=4, F=256):
        self.B, self.S, self.depth = B, S, depth
        self.G, self.EPG, self.E, self.F = G, EPG, G * EPG, F
        self.RPG = 4
        self.NCORE = B * self.RPG
        self.T = S // self.RPG
        self.NBLK = self.T // 128
        self.NM = self.NBLK // 2
        assert self.NBLK % 2 == 0 and self.NM >= 1
        self.ADAW = 6 * D // self.RPG
        self.groups = [[b * 4 + r for r in range(4)] for b in range(B)]
        self.FOX_IN = 3 * D + H
        self.NR = G + self.E
        self.NRP = 32


def seq_block(r, l):
    m, t = divmod(l, 2)
    return 8 * m + (r if t == 0 else 7 - r)


def dap(t, offset, ap):
    return bass.AP(tensor=t, offset=offset, ap=ap)


def build_program(cfg):
    nc = bass.Bass("TRN2", target_bir_lowering=False)
    T, NBLK, NM, E, G, EPG, F = cfg.T, cfg.NBLK, cfg.NM, cfg.E, cfg.G, cfg.EPG, cfg.F
    S = cfg.S
    depth = cfg.depth
    NR = cfg.NR
    NRP = cfg.NRP
    ADAW = cfg.ADAW

    def din(name, shape, dt=F32):
        return nc.dram_tensor(name, list(shape), dt, kind="ExternalInput")

    x_d = din("x", [T, D])
    c_d = din("c", [1, D])
    pos_d = din("pos", [128, NBLK], I32)
    qidx_d = din("qidx", [1, 256])
    kidx_d = din("kidx", [128, 8])
    sel_d = din("sel", [128, 4])
    invf_d = din("invf", [128, 16])
    wada_d = din("w_ada", [depth, D, ADAW])
    bada_d = din("b_ada", [depth, ADAW])
    mixn_d = din("mix_norm", [depth, D])
    ffnn_d = din("ffn_norm", [depth, D])
    fox_win_d = din("fox_w_in", [D, cfg.FOX_IN])
    fox_bf_d = din("fox_b_f", [1, H])
    fox_qn_d = din("fox_q_norm", [1, 64])
    fox_kn_d = din("fox_k_norm", [1, 64])
    fox_wout_d = din("fox_w_out", [D, D])
    if depth > 1:
        mla_win_d = din("mla_w_in", [D, 672])
        mla_qln_d = din("mla_q_lat_norm", [1, 384])
        mla_kvln_d = din("mla_kv_lat_norm", [1, 256])
        mla_wuq_d = din("mla_w_uq", [384, 1536])
        mla_wukv_d = din("mla_w_ukv", [256, 2048])
        mla_qn_d = din("mla_q_norm", [1, 96])
        mla_kn_d = din("mla_k_norm", [1, 96])
        mla_wout_d = din("mla_w_out", [D, D])
    wrt_d = din("moe_w_r", [depth, D, NRP])
    brt_d = din("moe_b_r", [depth, NRP])
    wg_d = din("moe_w_gate", [depth * E, D, F])
    wu_d = din("moe_w_up", [depth * E, D, F])
    wd_d = din("moe_w_down", [depth * E, F, D])
    y_d = nc.dram_tensor("y", [T, D], F32, kind="ExternalOutput")

    DKMAX = 96
    mod_src = nc.dram_tensor("mod_src", [depth, ADAW], F32)
    mod_dst = nc.dram_tensor("mod_dst", [4 * depth, ADAW], F32)
    lf_src = nc.dram_tensor("lf_src", [128, NBLK * H], F32)
    lf_dst = nc.dram_tensor("lf_dst", [4 * 128, NBLK * H], F32)
    qT_d = [nc.dram_tensor(f"qT_{li}", [H, DKMAX, T], BF16) for li in range(depth)]
    ROWS = {0: 64 + 3 + 65, 1: 96 + 65}
    kv_src = [[nc.dram_tensor(f"kvs_{li}_{h}", [ROWS[li], T], BF16) for h in range(H)] for li in range(depth)]
    kv_dst = [[nc.dram_tensor(f"kvd_{li}_{h}", [4 * ROWS[li], T], BF16) for h in range(H)] for li in range(depth)]

    with contextlib.ExitStack() as st:
        P = Prog(nc, st)

        uniq = {"n": 0}

        def sb(name, shape, dt=F32, stack=st):
            uniq["n"] += 1
            return stack.enter_context(nc.sbuf_tensor(f"{name}_u{uniq['n']}", list(shape), dt))

        x_sb = sb("x_sb", [128, NBLK, D])
        x_r = [Res(f"x{l}") for l in range(NBLK)]
        identf = sb("identf", [128, 128])
        identb = sb("identb", [128, 128], BF16)
        onesf = sb("onesf", [128, 128])
        trif = sb("trif", [128, 128])
        zerob = sb("zerob", [128, 256], BF16)
        maskb = sb("maskb", [128, 8, 256], BF16)
        sel_sb = sb("sel_sb", [128, 4])
        modbc = sb("modbc", [128, 6 * D])
        r_const = Res("const")
        r_mod = Res("modbc")
        r_moddst = Res("mod_dst")

        pbig = [st.enter_context(nc.psum_tensor(f"pp{i}", [128, 1024], F32)) for i in range(4)]
        psum = [pbig[i // 2][:, (i % 2) * 512:(i % 2 + 1) * 512] for i in range(8)]
        ps_r = [Res(f"ps{i}", psum=True) for i in range(8)]

        P.op("pool", lambda e: e.memset(onesf[:], 1.0), writes=[r_const], signal=False)
        P.op("pool", lambda e: e.memset(identf[:], 1.0), writes=[r_const], signal=False)
        P.op("pool", lambda e: e.memset(trif[:], 1.0), writes=[r_const], signal=False)
        P.op("pool", lambda e: e.memset(zerob[:], 0.0), writes=[r_const])
        P.op("pool", lambda e: e.affine_select(out=identf[:], in_=identf[:], pattern=[[-1, 128]],
                                               compare_op=ALU.is_equal, fill=0.0, base=0, channel_multiplier=1),
             reads=[r_const], writes=[r_const])
        P.op("pool", lambda e: e.affine_select(out=trif[:], in_=trif[:], pattern=[[1, 128]],
                                               compare_op=ALU.is_ge, fill=0.0, base=0, channel_multiplier=-1),
             reads=[r_const], writes=[r_const])
        P.op("dve", lambda e: e.tensor_copy(out=identb[:], in_=identf[:]), reads=[r_const], writes=[r_const])
        P.dma("sp", sel_sb[:], sel_d[:, :], "ldc", writes=[r_const])
        with contextlib.ExitStack() as s0:
            qib = sb("qib", [128, 256], stack=s0)
            kix = sb("kix", [128, 8], stack=s0)
            mtmp = sb("mtmp", [128, 256], stack=s0)
            cT = sb("cT", [128, 8], stack=s0)
            cS = sb("cS", [128, 8], stack=s0)
            cbc = sb("cbc", [128, 8, 128], stack=s0)
            wadas = [sb(f"wada{i}", [128, 8, ADAW], stack=s0) for i in range(min(depth, 2))]
            badat = sb("badat", [1, ADAW], stack=s0)
            modrow = badat
            r_t = Res("p0tmp")
            r_wadas = [Res(f"wada{i}") for i in range(2)]
            r_row = Res("modrow")
            r_bad = Res("badat")
            r_modsrc = Res("mod_src")
            P.dma("sp", qib[:], qidx_d[0:1, :].partition_broadcast(128), "ldc", writes=[r_t])
            P.dma("sp", kix[:], kidx_d[:, :], "ldc", writes=[r_t])
            P.dma("sp", cT[:], c_d.ap().rearrange("o (kc p) -> p (o kc)", p=128), "ldc", writes=[r_t],
                  allow_slow_non_contiguous=True)
            for j in range(8):
                P.op("dve", lambda e, j=j: e.tensor_scalar(out=mtmp[:], in0=qib[:], scalar1=kix[:, j:j + 1],
                                                          scalar2=None, op0=ALU.is_ge),
                     reads=[r_t], writes=[r_t])
                P.op("dve", lambda e, j=j: e.tensor_scalar(out=maskb[:, j, :], in0=mtmp[:], scalar1=-1.0,
                                                          scalar2=-NEG, op0=ALU.add, op1=ALU.mult),
                     reads=[r_t], writes=[r_const])
            P.op("act", lambda e: e.activation(out=cS[:], in_=cT[:], func=ACT.Silu), reads=[r_t], writes=[r_t])
            for kc in range(8):
                P.op("dve", lambda e, kc=kc: e.tensor_scalar(out=cbc[:, kc, :], in0=onesf[:], scalar1=cS[:, kc:kc + 1],
                                                            scalar2=None, op0=ALU.mult),
                     reads=[r_t, r_const], writes=[r_t])
            for li in range(depth):
                P.dma("sp", wadas[li % 2][:], wada_d[li].rearrange("(kc p) n -> p kc n", p=128), "ldw",
                      writes=[r_wadas[li % 2]])
            for l in range(NBLK):
                P.dma("sp", x_sb[:, l, :], x_d[l * 128:(l + 1) * 128, :], f"ldx{l % 4}", writes=[x_r[l]])
            for li in range(depth):
                wada, r_wada = wadas[li % 2], r_wadas[li % 2]
                P.dma("sp", badat[:], bada_d[li:li + 1, :], "ldw", writes=[r_bad])
                nch = (ADAW + 511) // 512
                for ch in range(nch):
                    c0, c1 = ch * 512, min(ADAW, (ch + 1) * 512)
                    bank = ch % 2
                    for kc in range(8):
                        P.op("pe", lambda e, kc=kc, c0=c0, c1=c1, bank=bank, wada=wada: e.matmul(
                            psum[bank][:, 0:c1 - c0], lhsT=cbc[:, kc, :], rhs=wada[:, kc, c0:c1],
                            start=(kc == 0), stop=(kc == 7)),
                            reads=[r_t, r_wada], writes=[ps_r[bank]], signal=(kc == 7))
                    P.op("dve", lambda e, c0=c0, c1=c1, bank=bank: e.tensor_tensor(
                        out=modrow[0:1, c0:c1], in0=psum[bank][0:1, 0:c1 - c0], in1=badat[0:1, c0:c1], op=ALU.add),
                        reads=[ps_r[bank], r_bad], writes=[r_bad])
                P.dma("sp", mod_src[li:li + 1, :], modrow[:], "stm", reads=[r_bad], writes=[r_modsrc], merge=(li > 0))
            P.collective(cfg.groups, mod_src.ap().opt(), mod_dst.ap().opt(), "cc_mod", reads=[r_modsrc],
                         writes=[r_moddst])
            P.barrier()

        def rstd_all(ss, rstd, r_ss, n):
            P.op("act", lambda e: e.activation(out=rstd, in_=ss, func=ACT.Sqrt, scale=1.0 / n, bias=EPS),
                 reads=[r_ss], writes=[r_ss])
            P.op("dve", lambda e: e.reciprocal(out=rstd, in_=rstd), reads=[r_ss], writes=[r_ss])

        def load_mod(li):
            P.dma("sp", modbc[:].rearrange("p (r n) -> p r n", r=4),
                  dap(mod_dst, li * ADAW, [[0, 128], [depth * ADAW, 4], [1, ADAW]]), "ldmod",
                  reads=[r_moddst], writes=[r_mod])

        def norm_mod(li, which, normvec_d, s1, tmpn, r_tmpn):
            off = 1 if which == 1 else 4
            P.dma("sp", tmpn[:], normvec_d[li:li + 1, :].partition_broadcast(128), "ldn", writes=[r_tmpn])
            P.op("dve", lambda e: e.scalar_tensor_tensor(out=modbc[:, off * D:(off + 1) * D],
                                                        in0=modbc[:, off * D:(off + 1) * D], scalar=1.0,
                                                        in1=tmpn[:], op0=ALU.add, op1=ALU.mult),
                 reads=[r_mod, r_tmpn], writes=[r_mod])

        def sumsq_blocks(ss, r_ss, junk, r_junk):
            for l in range(NBLK):
                P.op("act", lambda e, l=l: e.activation(out=junk[:], in_=x_sb[:, l, :], func=ACT.Square,
                                                       accum_out=ss[:, l:l + 1]),
                     reads=[x_r[l]], writes=[r_junk, r_ss])

        def make_hT(l, off_sh, off_A, rstd, r_ss, h32, r_h32, hT_out, r_hT, tb, hT32=None, r_hT32=None):
            P.op("dve", lambda e: e.scalar_tensor_tensor(out=h32[:], in0=x_sb[:, l, :], scalar=rstd[:, l:l + 1],
                                                        in1=modbc[:, off_A * D:(off_A + 1) * D],
                                                        op0=ALU.mult, op1=ALU.mult),
                 reads=[x_r[l], r_ss, r_mod], writes=[r_h32])
            P.op("pool", lambda e: e.tensor_tensor(out=h32[:], in0=h32[:], in1=modbc[:, off_sh * D:(off_sh + 1) * D],
                                                  op=ALU.add),
                 reads=[r_h32, r_mod], writes=[r_h32])
            yield
            for half in range(2):
                bank = tb[half]
                for k4 in range(4):
                    kc = half * 4 + k4
                    P.op("pe", lambda e, kc=kc, k4=k4, bank=bank: e.transpose(
                        out=psum[bank][:, k4 * 128:(k4 + 1) * 128], in_=h32[:, kc * 128:(kc + 1) * 128],
                        identity=identf[:]),
                        reads=[r_h32, r_const], writes=[ps_r[bank]], signal=(k4 == 3))
                P.op("act", lambda e, half=half, bank=bank: e.activation(
                    out=hT_out[:, half * 4:(half + 1) * 4, :], in_=psum[bank][:].rearrange("p (k t) -> p k t", k=4),
                    func=ACT.Copy),
                    reads=[ps_r[bank]], writes=[r_hT])
                if hT32 is not None:
                    P.op("dve", lambda e, half=half, bank=bank: e.tensor_copy(
                        out=hT32[:, half * 4:(half + 1) * 4, :],
                        in_=psum[bank][:].rearrange("p (k t) -> p k t", k=4)),
                        reads=[ps_r[bank]], writes=[r_hT32])
                yield

        def run_interleaved(gens):
            gens = list(gens)
            while gens:
                for g_ in list(gens):
                    try:
                        next(g_)
                    except StopIteration:
                        gens.remove(g_)

        def attention(li, dk, KA, scale, o_all, r_o):
            rows = ROWS[li]
            with contextlib.ExitStack() as sa:
                KT = [sb(f"KT{i}", [KA, 4 * T], BF16, stack=sa) for i in range(2)]
                VV = [sb(f"VV{i}", [128, 4 * NBLK, 65], BF16, stack=sa) for i in range(2)]
                QT = [sb(f"QT{i}", [KA, T], BF16, stack=sa) for i in range(2)]
                PT = [sb(f"PT{i}", [128, 1024], BF16, stack=sa) for i in range(3)]
                rec = sb("rec", [128, 2], stack=sa)
                r_KT = [Res(f"KT{i}") for i in range(2)]
                r_VV = [Res(f"VV{i}") for i in range(2)]
                r_QT = [Res(f"QT{i}") for i in range(2)]
                r_PT = [Res(f"PT{i}") for i in range(3)]
                r_rec = Res("rec")
                SBIG = [pbig[0], pbig[1], pbig[2]]
                r_S = [[ps_r[0], ps_r[1]], [ps_r[2], ps_r[3]], [ps_r[4], ps_r[5]]]
                OB = [6, 6]
                TB = [7, 7]
                osb = [sb(f"osb{i}", [65, 256], stack=sa) for i in range(2)]
                r_osb = [Res(f"osb{i}") for i in range(2)]
                pending = []
                fin_ctr = {"i": 0}
                if li == 0:
                    for i in range(2):
                        P.op("pool", lambda e, i=i: e.memset(KT[i][64:70, :], -1.0), writes=[r_KT[i]])
                        P.op("pool", lambda e, i=i: e.memset(QT[i][64:70, :], 1.0), writes=[r_QT[i]])

                def load_head(h):
                    b = h % 2
                    kd = kv_dst[li][h]
                    nk = dk + 3 if li == 0 else dk
                    P.dma("sp", KT[b][0:nk, :].rearrange("p (r t) -> p r t", r=4),
                          dap(kd, 0, [[T, nk], [rows * T, 4], [1, T]]), f"ldk{b}",
                          reads=[r_kvd[h]], writes=[r_KT[b]])
                    P.dma("sp", VV[b][:].rearrange("p (r l) c -> p r (l c)", r=4),
                          dap(kd, (rows - 65) * T, [[NBLK * 65, 128], [rows * T, 4], [1, NBLK * 65]]), f"ldv{b}",
                          reads=[r_kvd[h]], writes=[r_VV[b]])
                    P.dma("sp", QT[b][0:dk, :], qT_d[li][h, 0:dk, :], f"ldq{b}", reads=[r_qTd], writes=[r_QT[b]])
                    if li == 0:
                        P.dma("sp", QT[b][67:70, :], kv_src[li][h][64:67, :], f"ldq{b}",
                              reads=[r_kvs[h]], writes=[r_QT[b]])

                items = []
                for h in range(H):
                    for m in range(NM):
                        units = [(mp, rp, tp, 0) for mp in range(m) for rp in range(4) for tp in range(2)]
                        units += [(m, rp, 0, 0) for rp in range(4)]
                        units += [(m, rp, 1, 128) for rp in range(4)]
                        prs, cur, off = [], [], 0
                        for (mp, rp, tp, q0) in units:
                            w_ = 256 - q0
                            if off + w_ > 1024:
                                prs.append(cur)
                                cur, off = [], 0
                            cur.append((mp, rp, tp, q0, off))
                            off += w_
                        prs.append(cur)
                        for pi, pr in enumerate(prs):
                            items.append((h, m, pi, pr, pi == 0, pi == len(prs) - 1))
                state = {"n": 0}

                def emit_exp_pv(idx):
                    h, m, pi, pr, first, last = items[idx]
                    b = h % 2
                    sti = idx % 3
                    pt = idx % 3
                    ob = OB[(h * NM + m) % 2]
                    n = sum(256 - q0 for (_, _, _, q0, _) in pr)
                    P.op("act", lambda e, sti=sti, pt=pt, n=n: e.activation(
                        out=PT[pt][:, 0:n], in_=SBIG[sti][:, 0:n], func=ACT.Exp, scale=scale),
                        reads=r_S[sti], writes=[r_PT[pt]])
                    nu = len(pr)
                    for u, (mp, rp, tp, q0, off) in enumerate(pr):
                        kb = rp * NBLK + 2 * mp + tp
                        st_ = first and u == 0
                        fin = last and u == nu - 1
                        assert not (st_ and q0)
                        P.op("pe", lambda e, q0=q0, off=off, kb=kb, ob=ob, pt=pt, b=b, st_=st_, fin=fin: e.matmul(
                            psum[ob][0:65, q0:256], lhsT=VV[b][:, kb, :],
                            rhs=PT[pt][:, off:off + 256 - q0], start=st_, stop=fin),
                            reads=[r_PT[pt], r_VV[b]], writes=[ps_r[ob]], signal=(u == nu - 1))
                    if pending:
                        finalize(*pending.pop())
                    if last:
                        ou = fin_ctr["i"] % 2
                        fin_ctr["i"] += 1
                        P.op("dve", lambda e, ob=ob, ou=ou: e.tensor_copy(out=osb[ou][:], in_=psum[ob][0:65, 0:256]),
                             reads=[ps_r[ob]], writes=[r_osb[ou]])
                        pending.append((h, m, ou))

                def finalize(h, m, ou):
                    tb = TB[ou]
                    for tq in range(2):
                        P.op("pe", lambda e, tq=tq, ou=ou, tb=tb: e.transpose(
                            out=psum[tb][:, tq * 65:(tq + 1) * 65], in_=osb[ou][:, tq * 128:(tq + 1) * 128],
                            identity=identf[0:65, 0:65]),
                            reads=[r_osb[ou], r_const], writes=[ps_r[tb]], signal=(tq == 1))
                    P.op("dve", lambda e, tb=tb: e.reciprocal(
                        out=rec[:], in_=psum[tb][:, 0:130].rearrange("p (t c) -> p t c", t=2)[:, :, 64]),
                        reads=[ps_r[tb]], writes=[r_rec])
                    for tq in range(2):
                        l = 2 * m + tq
                        P.op("dve", lambda e, tq=tq, l=l, tb=tb, h=h: e.tensor_scalar(
                            out=o_all[:, l, h * 64:(h + 1) * 64], in0=psum[tb][:, tq * 65:tq * 65 + 64],
                            scalar1=rec[:, tq:tq + 1], scalar2=None, op0=ALU.mult),
                            reads=[ps_r[tb], r_rec], writes=[r_o])

                def emit_qk_sig(idx):
                    h, m, pi, pr, first, last = items[idx]
                    b = h % 2
                    sti = idx % 3
                    ninst = sum(2 if mp == m else 1 for (mp, rp, tp, q0, off) in pr)
                    k = 0
                    for u, (mp, rp, tp, q0, off) in enumerate(pr):
                        kcol = rp * T + (2 * mp + tp) * 128
                        diag = (mp == m)
                        w_ = 256 - q0
                        k += 1
                        P.op("pe", lambda e, off=off, w_=w_, q0=q0, kcol=kcol, diag=diag, sti=sti, b=b, m=m: e.matmul(
                            SBIG[sti][:, off:off + w_], lhsT=KT[b][:, kcol:kcol + 128],
                            rhs=QT[b][:, m * 256 + q0:(m + 1) * 256], start=True, stop=(not diag)),
                            reads=[r_KT[b], r_QT[b]], writes=r_S[sti], signal=(k == ninst))
                        if diag:
                            j = rp * 2 + tp
                            k += 1
                            P.op("pe", lambda e, off=off, w_=w_, q0=q0, j=j, sti=sti: e.matmul(
                                SBIG[sti][:, off:off + w_], lhsT=identb[:], rhs=maskb[:, j, q0:256],
                                start=False, stop=True),
                                reads=[r_const], writes=r_S[sti], signal=(k == ninst))

                loaded = set()

                def ensure_loaded(h):
                    if h < H and h not in loaded:
                        loaded.add(h)
                        load_head(h)

                ensure_loaded(0)
                ensure_loaded(1)
                LOOK = 2
                nit = len(items)
                for idx in range(min(LOOK, nit)):
                    emit_qk_sig(idx)
                for idx in range(nit):
                    emit_exp_pv(idx)
                    if idx + LOOK < nit:
                        hn = items[idx + LOOK][0]
                        emit_qk_sig(idx + LOOK)
                    h, m, pi, pr, first, last = items[idx]
                    if last and m == NM - 1:
                        ensure_loaded(h + 2)
                while pending:
                    finalize(*pending.pop())
                P.barrier()

        def out_proj(li, wout_d, o_all, r_o):
            with contextlib.ExitStack() as so:
                wo = sb("wo", [128, 8, D], BF16, stack=so)
                oT = [sb(f"oT{i}", [128, 8, 128], BF16, stack=so) for i in range(2)]
                r_wo = Res("wo")
                r_oT = [Res(f"oT{i}") for i in range(2)]
                P.dma("pool", wo[:], wout_d.ap().rearrange("(kc p) n -> p kc n", p=128), "ldwo", writes=[r_wo])
                for kc in range(8):
                    P.op("dve", lambda e, kc=kc: e.tensor_tensor(out=wo[:, kc, :], in0=wo[:, kc, :],
                                                                in1=modbc[:, 2 * D:3 * D], op=ALU.mult),
                         reads=[r_wo, r_mod], writes=[r_wo])
                def emit_T(l):
                    b = l % 2
                    tb = 5 + b
                    pT = psum[tb][:].bitcast(BF16)
                    for kc in range(8):
                        P.op("pe", lambda e, kc=kc, l=l, pT=pT: e.transpose(
                            out=pT[:, kc * 128:(kc + 1) * 128], in_=o_all[:, l, kc * 128:(kc + 1) * 128],
                            identity=identb[:]),
                            reads=[r_o, r_const], writes=[ps_r[tb]], signal=(kc == 7))
                    P.op("act", lambda e, b=b, pT=pT: e.activation(
                        out=oT[b][:], in_=pT[:, 0:1024].rearrange("p (k t) -> p k t", k=8), func=ACT.Copy),
                        reads=[ps_r[tb]], writes=[r_oT[b]])

                def emit_MM(l):
                    b = l % 2
                    yb = 0 if b == 0 else 2
                    for half in range(2):
                        for kc in range(8):
                            P.op("pe", lambda e, kc=kc, half=half, b=b, yb=yb: e.matmul(
                                psum[yb + half][:, :], lhsT=oT[b][:, kc, :], rhs=wo[:, kc, half * 512:(half + 1) * 512],
                                start=(kc == 0), stop=(kc == 7)),
                                reads=[r_oT[b], r_wo], writes=[ps_r[yb + half]], signal=(kc == 7))
                        P.op("dve", lambda e, half=half, l=l, yb=yb: e.tensor_tensor(
                            out=x_sb[:, l, half * 512:(half + 1) * 512], in0=x_sb[:, l, half * 512:(half + 1) * 512],
                            in1=psum[yb + half][:, :], op=ALU.add),
                            reads=[ps_r[yb + half], x_r[l]], writes=[x_r[l]])

                emit_T(0)
                for l in range(NBLK):
                    if l + 1 < NBLK:
                        emit_T(l + 1)
                    emit_MM(l)
                P.barrier()

        def moe(li):
            with contextlib.ExitStack() as sm:
                hT2 = sb("hT2", [128, NBLK, 8, 128], BF16, stack=sm)
                r_hT2 = [Res(f"hT2_{l}") for l in range(NBLK)]
                comb = sb("comb", [128, NBLK, E], stack=sm)
                r_comb = Res("comb")
                ss = sb("ss2", [128, NBLK], stack=sm)
                rstd = sb("rstd2", [128, NBLK], stack=sm)
                r_ss = Res("ss2")
                wgu = [sb("wgu0", [128, 8, 2, 2 * F], BF16, stack=sm), None]
                wdn = [sb("wdn0", [128, 2, 2, D], BF16, stack=sm), None]
                r_wgu = [Res(f"wgu{i}") for i in range(2)]
                r_wdn = [Res(f"wdn{i}") for i in range(2)]

                def load_pair_dma(ep):
                    b = ep % 2
                    for e2 in range(2):
                        ee = li * E + ep * 2 + e2
                        P.dma("pool", wgu[b][:, :, e2, 0:F], wg_d[ee].rearrange("(kc p) f -> p kc f", p=128),
                              writes=[r_wgu[b]], merge=(e2 > 0))
                        P.dma("pool", wgu[b][:, :, e2, F:2 * F], wu_d[ee].rearrange("(kc p) f -> p kc f", p=128),
                              writes=[r_wgu[b]], merge=True)
                        P.dma("pool", wdn[b][:, e2, :, :], wd_d[ee].rearrange("(fc p) d -> p fc d", p=128),
                              writes=[r_wdn[b]], merge=(e2 > 0))

                def load_pair_scale(ep):
                    b = ep % 2
                    for e2 in range(2):
                        for fc in range(2):
                            P.op("pool", lambda e, e2=e2, fc=fc, b=b: e.tensor_tensor(
                                out=wdn[b][:, e2, fc, :], in0=wdn[b][:, e2, fc, :], in1=modbc[:, 5 * D:6 * D],
                                op=ALU.mult),
                                reads=[r_wdn[b], r_mod], writes=[r_wdn[b]])

                def load_pair(ep):
                    load_pair_dma(ep)
                    load_pair_scale(ep)

                load_pair_dma(0)
                with contextlib.ExitStack() as s1:
                    junk = sb("junk2", [128, D], stack=s1)
                    r_junk = Res("junk2")
                    tmpn = sb("tmpn2", [128, D], stack=s1)
                    r_tmpn = Res("tmpn2")
                    h32 = [sb(f"h32b_{i}", [128, D], stack=s1) for i in range(2)]
                    r_h32 = [Res(f"h32b_{i}") for i in range(2)]
                    hT32 = [sb(f"hT32_{i}", [128, 8, 128], stack=s1) for i in range(2)]
                    r_hT32 = [Res(f"hT32_{i}") for i in range(2)]
                    wr = sb("wr", [128, 8, NRP], stack=s1)
                    brb = sb("brb", [128, NRP], stack=s1)
                    r_wr = Res("wr")
                    LG = sb("LG", [128, NBLK, NRP], stack=s1)
                    r_LG = Res("LG")
                    if "wr" not in os.environ.get("MK_SKIP", ""):
                        P.dma("sp", wr[:], wrt_d[li].rearrange("(kc p) n -> p kc n", p=128), "ldwr", writes=[r_wr])
                    if "br" not in os.environ.get("MK_SKIP", ""):
                        P.dma("sp", brb[:], brt_d[li:li + 1, :].partition_broadcast(128), "ldwr", writes=[r_wr])
                    norm_mod(li, 2, ffnn_d, None, tmpn, r_tmpn)
                    sumsq_blocks(ss, r_ss, junk, r_junk)
                    rstd_all(ss[:], rstd[:], r_ss, D)
                    def pre_blk(l):
                        b = l % 2
                        yield from make_hT(l, 3, 4, rstd, r_ss, h32[b], r_h32[b], hT2[:, l], r_hT2[l],
                                           (0 + 2 * b, 1 + 2 * b), hT32=hT32[b], r_hT32=r_hT32[b])
                        lb = 4 + b
                        for kc in range(8):
                            P.op("pe", lambda e, kc=kc, b=b, lb=lb: e.matmul(
                                psum[lb][:, 0:NRP], lhsT=hT32[b][:, kc, :], rhs=wr[:, kc, :],
                                start=(kc == 0), stop=(kc == 7)),
                                reads=[r_hT32[b], r_wr], writes=[ps_r[lb]], signal=(kc == 7))
                        P.op("dve", lambda e, l=l, lb=lb: e.tensor_tensor(
                            out=LG[:, l, :], in0=psum[lb][:, 0:NRP], in1=brb[:], op=ALU.add),
                            reads=[ps_r[lb], r_wr], writes=[r_LG], extra=())
                        yield

                    for l in range(0, NBLK, 2):
                        run_interleaved([pre_blk(l), pre_blk(l + 1)])
                    if stop_point("logits", P):
                        return
                    gl = LG[:, :, 0:G]
                    el = LG[:, :, G:NR].rearrange("p l (g e) -> p l g e", g=G)
                    gmax = sb("gmax", [128, NBLK], stack=s1)
                    gexp = sb("gexp", [128, NBLK, G], stack=s1)
                    gsum = sb("gsum", [128, NBLK], stack=s1)
                    gm = sb("gm", [128, NBLK, G], stack=s1)
                    esel = sb("esel", [128, NBLK, G, EPG], stack=s1)
                    ing = sb("ing", [128, NBLK, EPG], stack=s1)
                    m1 = sb("m1", [128, NBLK], stack=s1)
                    k1 = sb("k1", [128, NBLK, EPG], stack=s1)
                    ing2 = sb("ing2", [128, NBLK, EPG], stack=s1)
                    m2 = sb("m2", [128, NBLK], stack=s1)
                    k2 = sb("k2", [128, NBLK, EPG], stack=s1)
                    w1 = sb("w1", [128, NBLK], stack=s1)
                    w2 = sb("w2", [128, NBLK], stack=s1)
                    cig = sb("cig", [128, NBLK, EPG], stack=s1)
                    r_rt = Res("rt")

                    def dv(fn, reads=(), writes=()):
                        P.op("dve", fn, reads=[r_LG, r_rt] + list(reads), writes=[r_rt] + list(writes))

                    def bcl(ap2, n):
                        return ap2.unsqueeze(2).to_broadcast([128, NBLK, n])

                    dv(lambda e: e.tensor_reduce(out=gmax[:], in_=gl, axis=AX.X, op=ALU.max))
                    dv(lambda e: e.tensor_tensor(out=gexp[:], in0=gl, in1=bcl(gmax[:], G), op=ALU.subtract))
                    dv(lambda e: e.tensor_tensor(out=gm[:], in0=gl, in1=bcl(gmax[:], G), op=ALU.is_ge))
                    P.op("act", lambda e: e.activation(out=gexp[:], in_=gexp[:], func=ACT.Exp),
                         reads=[r_rt], writes=[r_rt])
                    dv(lambda e: e.tensor_reduce(out=gsum[:], in_=gexp[:], axis=AX.X, op=ALU.add))
                    dv(lambda e: e.reciprocal(out=gsum[:], in_=gsum[:]))
                    dv(lambda e: e.tensor_tensor(out=esel[:], in0=el,
                                                 in1=gm[:].unsqueeze(3).to_broadcast([128, NBLK, G, EPG]),
                                                 op=ALU.mult))
                    dv(lambda e: e.tensor_reduce(out=ing[:], in_=esel[:].rearrange("p l g e -> p l e g"),
                                                 axis=AX.X, op=ALU.add))
                    dv(lambda e: e.tensor_reduce(out=m1[:], in_=ing[:], axis=AX.X, op=ALU.max))
                    dv(lambda e: e.tensor_tensor(out=k1[:], in0=ing[:], in1=bcl(m1[:], EPG), op=ALU.is_ge))
                    dv(lambda e: e.scalar_tensor_tensor(out=ing2[:], in0=k1[:], scalar=-1e30, in1=ing[:],
                                                        op0=ALU.mult, op1=ALU.add))
                    dv(lambda e: e.tensor_reduce(out=m2[:], in_=ing2[:], axis=AX.X, op=ALU.max))
                    dv(lambda e: e.tensor_tensor(out=k2[:], in0=ing2[:], in1=bcl(m2[:], EPG), op=ALU.is_ge))
                    dv(lambda e: e.tensor_tensor(out=w1[:], in0=m2[:], in1=m1[:], op=ALU.subtract))
                    P.op("act", lambda e: e.activation(out=w1[:], in_=w1[:], func=ACT.Exp), reads=[r_rt], writes=[r_rt])
                    dv(lambda e: e.tensor_scalar(out=w1[:], in0=w1[:], scalar1=1.0, scalar2=None, op0=ALU.add))
                    dv(lambda e: e.reciprocal(out=w1[:], in_=w1[:]))
                    dv(lambda e: e.tensor_scalar(out=w2[:], in0=w1[:], scalar1=-1.0, scalar2=1.0, op0=ALU.mult,
                                                 op1=ALU.add))
                    dv(lambda e: e.tensor_tensor(out=w1[:], in0=w1[:], in1=gsum[:], op=ALU.mult))
                    dv(lambda e: e.tensor_tensor(out=w2[:], in0=w2[:], in1=gsum[:], op=ALU.mult))
                    dv(lambda e: e.tensor_tensor(out=k1[:], in0=k1[:], in1=bcl(w1[:], EPG), op=ALU.mult))
                    dv(lambda e: e.tensor_tensor(out=k2[:], in0=k2[:], in1=bcl(w2[:], EPG), op=ALU.mult))
                    dv(lambda e: e.tensor_tensor(out=cig[:], in0=k1[:], in1=k2[:], op=ALU.add))
                    dv(lambda e: e.tensor_tensor(out=comb[:].rearrange("p l (g e) -> p l g e", g=G),
                                                 in0=gm[:].unsqueeze(3).to_broadcast([128, NBLK, G, EPG]),
                                                 in1=cig[:].unsqueeze(2).to_broadcast([128, NBLK, G, EPG]),
                                                 op=ALU.mult), writes=[r_comb])
                    P.barrier()
                if stop_point("route", P):
                    return
                with contextlib.ExitStack() as s2:
                    wgu[1] = sb("wgu1", [128, 8, 2, 2 * F], BF16, stack=s2)
                    wdn[1] = sb("wdn1", [128, 2, 2, D], BF16, stack=s2)
                    sa_t = [sb(f"sa{i}", [128, F], stack=s2) for i in range(2)]
                    hid = [sb(f"hid{i}", [128, F], BF16, stack=s2) for i in range(2)]
                    hidT = [sb(f"hidT{i}", [128, 2, 128], BF16, stack=s2) for i in range(2)]
                    r_sa = [Res(f"sa{i}") for i in range(2)]
                    r_hid = [Res(f"hid{i}") for i in range(2)]
                    r_hidT = [Res(f"hidT{i}") for i in range(2)]
                    NP = E // 2
                    load_pair_scale(0)
                    if stop_point("wload", P):
                        return
                    units = [(ep, l, e2) for ep in range(NP) for l in range(NBLK) for e2 in range(2)]
                    NU = len(units)
                    GUB = [0, 1, 2]
                    TTB = 3

                    def emit_gu(i):
                        ep, l, e2 = units[i]
                        b = ep % 2
                        gub = GUB[i % 3]
                        for kc in range(8):
                            P.op("pe", lambda e, kc=kc, l=l, e2=e2, b=b, gub=gub: e.matmul(
                                psum[gub][:, :], lhsT=hT2[:, l, kc, :], rhs=wgu[b][:, kc, e2, :],
                                start=(kc == 0), stop=(kc == 7)),
                                reads=[r_hT2[l], r_wgu[b]], writes=[ps_r[gub]], signal=(kc == 7))

                    def emit_rest(i):
                        ep, l, e2 = units[i]
                        b = ep % 2
                        ee = ep * 2 + e2
                        g = i % 2
                        gub = GUB[i % 3]
                        yb = 4 + 2 * (l % 2)
                        P.op("act", lambda e, g=g, gub=gub: e.activation(
                            out=sa_t[g][:], in_=psum[gub][:, 0:F], func=ACT.Silu),
                            reads=[ps_r[gub]], writes=[r_sa[g]])
                        P.op("dve", lambda e, g=g, gub=gub, l=l, ee=ee: e.scalar_tensor_tensor(
                            out=hid[g][:], in0=psum[gub][:, F:2 * F], scalar=comb[:, l, ee:ee + 1],
                            in1=sa_t[g][:], op0=ALU.mult, op1=ALU.mult),
                            reads=[ps_r[gub], r_comb, r_sa[g]], writes=[r_hid[g]])
                        pT = psum[TTB][:].bitcast(BF16)
                        for fc in range(2):
                            P.op("pe", lambda e, fc=fc, g=g, pT=pT: e.transpose(
                                out=pT[:, fc * 128:(fc + 1) * 128], in_=hid[g][:, fc * 128:(fc + 1) * 128],
                                identity=identb[:]),
                                reads=[r_hid[g], r_const], writes=[ps_r[TTB]], signal=(fc == 1))
                        if i + 2 < NU:
                            emit_gu(i + 2)
                        P.op("act", lambda e, g=g, pT=pT: e.activation(
                            out=hidT[g][:], in_=pT[:, 0:256].rearrange("p (f t) -> p f t", f=2),
                            func=ACT.Copy),
                            reads=[ps_r[TTB]], writes=[r_hidT[g]])
                        for fc in range(2):
                            for half in range(2):
                                first = (e2 == 0 and fc == 0)
                                lastm = (e2 == 1 and fc == 1)
                                P.op("pe", lambda e, fc=fc, half=half, g=g, b=b, e2=e2, yb=yb, first=first,
                                     lastm=lastm: e.matmul(
                                    psum[yb + half][:, :], lhsT=hidT[g][:, fc, :],
                                    rhs=wdn[b][:, e2, fc, half * 512:(half + 1) * 512],
                                    start=first, stop=lastm),
                                    reads=[r_hidT[g], r_wdn[b]], writes=[ps_r[yb + half]],
                                    signal=(fc == 1 and half == 1))
                        if e2 == 1:
                            for half in range(2):
                                P.op("dve", lambda e, half=half, l=l, yb=yb: e.tensor_tensor(
                                    out=x_sb[:, l, half * 512:(half + 1) * 512],
                                    in0=x_sb[:, l, half * 512:(half + 1) * 512], in1=psum[yb + half][:, :],
                                    op=ALU.add),
                                    reads=[ps_r[yb + half], x_r[l]], writes=[x_r[l]])

                    emit_gu(0)
                    if NU > 1:
                        emit_gu(1)
                    for i in range(NU):
                        ep, l, e2 = units[i]
                        if l == 0 and e2 == 0 and ep + 1 < NP:
                            load_pair(ep + 1)
                        emit_rest(i)
                    P.barrier()

        r_qTd = Res("qTd")
        r_kvs = [Res(f"kvs{h}") for h in range(H)]
        r_kvd = [Res(f"kvd{h}") for h in range(H)]

        def fox_layer(li):
            load_mod(li)
            with contextlib.ExitStack() as sl:
                lf = sb("lf", [128, NBLK, H], stack=sl)
                r_lf = Res("lf")
                with contextlib.ExitStack() as s1:
                    win = sb("win", [128, 8, cfg.FOX_IN], BF16, stack=s1)
                    r_win = [Res(f"win{ci}") for ci in range(7)]
                    ss = sb("ss", [128, NBLK], stack=s1)
                    rstd = sb("rstd", [128, NBLK], stack=s1)
                    r_ss = Res("ss")
                    h32 = [sb(f"h32_{i}", [128, D], stack=s1) for i in range(2)]
                    r_h32 = [Res(f"h32_{i}") for i in range(2)]
                    hT = [sb(f"hT_{i}", [128, 8, 128], BF16, stack=s1) for i in range(2)]
                    r_hT = [Res(f"hT_{i}") for i in range(2)]
                    qk32s = [[sb(f"qk32_{j}_{i}", [128, D], stack=s1) for i in range(2)] for j in range(2)]
                    r_qk32s = [[Res(f"qk32_{j}_{i}") for i in range(2)] for j in range(2)]
                    qk32, r_qk32 = qk32s[0], r_qk32s[0]
                    sq = sb("sq", [128, D], stack=s1)
                    r_sq = Res("sq")
                    ssqs = [sb(f"ssq{i}", [128, 2, H], stack=s1) for i in range(2)]
                    r_ssqs = [Res(f"ssq{i}") for i in range(2)]
                    rsq = sb("rsq", [128, 2, H], stack=s1)
                    r_rsq = Res("rsq")
                    gq = sb("gq", [128, 2, 64], stack=s1)
                    r_gq = Res("gq")
                    qkn = [sb(f"qkn_{i}", [128, D], BF16, stack=s1) for i in range(2)]
                    r_qkn = [Res(f"qkn_{i}") for i in range(2)]
                    qkT = [sb(f"qkT_{i}", [64, H, 128], BF16, stack=s1) for i in range(2)]
                    r_qkT = [Res(f"qkT_{i}") for i in range(2)]
                    vsts = [sb(f"vst{i}", [128, H, 65], BF16, stack=s1) for i in range(2)]
                    r_vsts = [Res(f"vst{i}") for i in range(2)]
                    bfb = sb("bfb", [128, H], stack=s1)
                    zfs = [sb(f"zf{i}", [128, H], stack=s1) for i in range(2)]
                    r_zfs = [Res(f"zf{i}") for i in range(2)]
                    tmpn, r_tmpn = sq, r_sq
                    junk, r_junk = qk32[0], r_qk32[0]

                    for ci in range(7):
                        c0_, c1_ = ci * 512, min(cfg.FOX_IN, ci * 512 + 512)
                        P.dma("pool", win[:, :, c0_:c1_],
                              fox_win_d.ap().rearrange("(kc p) n -> p kc n", p=128)[:, :, c0_:c1_], writes=[r_win[ci]])
                    P.dma("sp", gq[:, 0, :], fox_qn_d[0:1, :].partition_broadcast(128), "ldn", writes=[r_gq])
                    P.dma("sp", gq[:, 1, :], fox_kn_d[0:1, :].partition_broadcast(128), "ldn", writes=[r_gq])
                    P.dma("sp", bfb[:], fox_bf_d[0:1, :].partition_broadcast(128), "ldn", writes=[r_gq])
                    for i_ in range(2):
                        P.op("pool", lambda e, i_=i_: e.memset(vsts[i_][:, :, 64:65], 1.0), writes=[r_vsts[i_]])
                    norm_mod(li, 1, mixn_d, None, tmpn, r_tmpn)
                    sumsq_blocks(ss, r_ss, junk, r_junk)
                    rstd_all(ss[:], rstd[:], r_ss, D)
                    def blockA(l):
                        b = l % 2
                        qk32, r_qk32, ssq, r_ssq = qk32s[b], r_qk32s[b], ssqs[b], r_ssqs[b]
                        vst, r_vst, zf, r_zf = vsts[b], r_vsts[b], zfs[b], r_zfs[b]
                        yield from make_hT(l, 0, 1, rstd, r_ss, h32[b], r_h32[b], hT[b], r_hT[b], (0, 1))
                        for ci in range(7):
                            c0 = ci * 512
                            c1 = min(cfg.FOX_IN, c0 + 512)
                            bank = 2 + (ci % 4)
                            for kc in range(8):
                                P.op("pe", lambda e, kc=kc, c0=c0, c1=c1, bank=bank, b=b: e.matmul(
                                    psum[bank][:, 0:c1 - c0], lhsT=hT[b][:, kc, :], rhs=win[:, kc, c0:c1],
                                    start=(kc == 0), stop=(kc == 7)),
                                    reads=[r_hT[b], r_win[ci]], writes=[ps_r[bank]], signal=(kc == 7))
                            if ci < 4:
                                w = ci // 2
                                hf = ci % 2
                                P.op("act", lambda e, w=w, hf=hf, bank=bank: e.activation(
                                    out=qk32[w][:, hf * 512:(hf + 1) * 512], in_=psum[bank][:, :], func=ACT.Copy),
                                    reads=[ps_r[bank]], writes=[r_qk32[w]])
                                P.op("act", lambda e, w=w, hf=hf, bank=bank: e.activation(
                                    out=sq[:, hf * 512:(hf + 1) * 512], in_=psum[bank][:, :], func=ACT.Square),
                                    reads=[ps_r[bank]], writes=[r_sq])
                                P.op("dve", lambda e, w=w, hf=hf: e.tensor_reduce(
                                    out=ssq[:, w, hf * 8:(hf + 1) * 8],
                                    in_=sq[:, hf * 512:(hf + 1) * 512].rearrange("p (h d) -> p h d", d=64),
                                    axis=AX.X, op=ALU.add),
                                    reads=[r_sq], writes=[r_ssq])
                            elif ci < 6:
                                hf = ci - 4
                                P.op("act", lambda e, hf=hf, bank=bank: e.activation(
                                    out=vst[:, hf * 8:(hf + 1) * 8, 0:64],
                                    in_=psum[bank][:, :].rearrange("p (h d) -> p h d", d=64), func=ACT.Copy),
                                    reads=[ps_r[bank]], writes=[r_vst])
                            else:
                                P.op("dve", lambda e, bank=bank: e.tensor_tensor(
                                    out=zf[:], in0=psum[bank][:, 0:H], in1=bfb[:], op=ALU.add),
                                    reads=[ps_r[bank], r_gq], writes=[r_zf])
                            yield

                    def blockB(l):
                        b = l % 2
                        qk32, r_qk32, ssq, r_ssq = qk32s[b], r_qk32s[b], ssqs[b], r_ssqs[b]
                        vst, r_vst, zf, r_zf = vsts[b], r_vsts[b], zfs[b], r_zfs[b]
                        P.op("act", lambda e: e.activation(out=zf[:], in_=zf[:], func=ACT.Exp, scale=-1.0),
                             reads=[r_zf], writes=[r_zf])
                        P.op("act", lambda e: e.activation(out=zf[:], in_=zf[:], func=ACT.Ln, bias=1.0),
                             reads=[r_zf], writes=[r_zf])
                        P.op("dve", lambda e, l=l: e.tensor_scalar(out=lf[:, l, :], in0=zf[:], scalar1=-1.0,
                                                                  scalar2=None, op0=ALU.mult),
                             reads=[r_zf], writes=[r_lf])
                        yield
                        P.op("act", lambda e: e.activation(out=rsq[:], in_=ssq[:], func=ACT.Sqrt, scale=1.0 / 64,
                                                           bias=EPS), reads=[r_ssq], writes=[r_rsq])
                        P.op("dve", lambda e: e.reciprocal(out=rsq[:], in_=rsq[:]), reads=[r_rsq], writes=[r_rsq])
                        yield
                        for w in range(2):
                            P.op("dve", lambda e, w=w: e.tensor_tensor(
                                out=qk32[w][:].rearrange("p (h d) -> p h d", d=64),
                                in0=qk32[w][:].rearrange("p (h d) -> p h d", d=64),
                                in1=rsq[:, w, :].unsqueeze(2).to_broadcast([128, H, 64]), op=ALU.mult),
                                reads=[r_qk32[w], r_rsq], writes=[r_qk32[w]])
                            yield
                            P.op("pool", lambda e, w=w: e.tensor_tensor(
                                out=qkn[w][:].rearrange("p (h d) -> p h d", d=64),
                                in0=qk32[w][:].rearrange("p (h d) -> p h d", d=64),
                                in1=gq[:, w, :].unsqueeze(1).to_broadcast([128, H, 64]), op=ALU.mult),
                                reads=[r_qk32[w], r_gq], writes=[r_qkn[w]])
                            yield
                            for hb in range(2):
                                tbk = 6 + hb
                                pT = psum[tbk][:].bitcast(BF16)
                                for h8 in range(8):
                                    hh = hb * 8 + h8
                                    P.op("pe", lambda e, w=w, hh=hh, h8=h8, pT=pT: e.transpose(
                                        out=pT[0:64, h8 * 128:(h8 + 1) * 128], in_=qkn[w][:, hh * 64:(hh + 1) * 64],
                                        identity=identb[:]),
                                        reads=[r_qkn[w], r_const], writes=[ps_r[tbk]], signal=(h8 == 7))
                                P.op("act", lambda e, w=w, hb=hb, pT=pT: e.activation(
                                    out=qkT[w][:, hb * 8:(hb + 1) * 8, :],
                                    in_=pT[0:64, 0:1024].rearrange("p (h t) -> p h t", h=8), func=ACT.Copy),
                                    reads=[ps_r[tbk]], writes=[r_qkT[w]])
                                yield
                        P.dma("sp", dap(qT_d[li], l * 128, [[T, 64], [DKMAX * T, H], [1, 128]]), qkT[0][:],
                              "stq", reads=[r_qkT[0]], writes=[r_qTd], merge=True)
                        for h in range(H):
                            P.dma("sp", kv_src[li][h][0:64, l * 128:(l + 1) * 128], qkT[1][:, h, :], "stk",
                                  reads=[r_qkT[1]], writes=[r_kvs[h]], merge=True)
                            P.dma("sp", dap(kv_src[li][h], 67 * T + l * 65, [[NBLK * 65, 128], [1, 65]]),
                                  vst[:, h, :], "stv", reads=[r_vst], writes=[r_kvs[h]], merge=True)
                        yield

                    def run_interleaved(gens):
                        gens = list(gens)
                        while gens:
                            for g_ in list(gens):
                                try:
                                    next(g_)
                                except StopIteration:
                                    gens.remove(g_)

                    run_interleaved([blockA(0)])
                    for l in range(NBLK):
                        gl_ = [blockA(l + 1)] if l + 1 < NBLK else []
                        run_interleaved(gl_ + [blockB(l)])
                    P.barrier()
                    if stop_point("proj", P):
                        return
                with contextlib.ExitStack() as s1:
                    r_lfs = Res("lf_src")
                    r_lfd = Res("lf_dst")
                    P.dma("sp", lf_src.ap(), lf[:].rearrange("p l h -> p (l h)"), "stlf", reads=[r_lf], writes=[r_lfs])
                    P.collective(cfg.groups, lf_src.ap().opt(), lf_dst.ap().opt(), "cc_lf", reads=[r_lfs],
                                 writes=[r_lfd])
                    NK = 4 * NBLK
                    L = sb("Lall", [128, NK, H], stack=s1)
                    W = sb("Wall", [128, NK, H], stack=s1)
                    tot = sb("tot", [128, H, 8 * NM], stack=s1)
                    pre = sb("pre", [128, H, 8 * NM], stack=s1)
                    onesrow = sb("onesrow", [128, 8 * NM], stack=s1)
                    ownc = sb("ownc", [128, NBLK, H], stack=s1)
                    c3 = sb("c3", [128, NBLK, H, 3], BF16, stack=s1)
                    r1 = sb("r1", [128, NBLK, H], stack=s1)
                    hi = sb("hi", [128, NBLK, H], BF16, stack=s1)
                    augT = sb("augT", [48, NBLK, 128], BF16, stack=s1)
                    r_L = Res("Lall")
                    r_cs = Res("cs")
                    r_aug = Res("augT")
                    P.dma("sp", L[:].rearrange("p (r l) h -> p r (l h)", r=4),
                          dap(lf_dst, 0, [[NBLK * H, 128], [128 * NBLK * H, 4], [1, NBLK * H]]), "ldlf",
                          reads=[r_lfd], writes=[r_L])
                    P.op("pool", lambda e: e.memset(onesrow[:], 1.0), writes=[r_cs])
                    Lf = L[:].rearrange("p k h -> p (k h)")
                    nch = (NK * H + 511) // 512
                    for ch in range(nch):
                        c0, c1 = ch * 512, min(NK * H, (ch + 1) * 512)
                        P.op("pe", lambda e, c0=c0, c1=c1: e.matmul(psum[0][:, 0:c1 - c0], lhsT=trif[:], rhs=Lf[:, c0:c1],
                                                                    start=True, stop=True),
                             reads=[r_L, r_const], writes=[ps_r[0]])
                        P.op("pe", lambda e, c0=c0, c1=c1: e.matmul(psum[1][:, 0:c1 - c0], lhsT=onesf[:], rhs=Lf[:, c0:c1],
                                                                    start=True, stop=True),
                             reads=[r_L, r_const], writes=[ps_r[1]])
                        P.op("dve", lambda e, c0=c0, c1=c1: e.tensor_copy(
                            out=W[:].rearrange("p k h -> p (k h)")[:, c0:c1], in_=psum[0][:, 0:c1 - c0]),
                            reads=[ps_r[0]], writes=[r_cs])
                        nkb = (c1 - c0) // H
                        for kk in range(nkb):
                            kbi = c0 // H + kk
                            rp, lp = divmod(kbi, NBLK)
                            sbk = seq_block(rp, lp)
                            P.op("dve", lambda e, kk=kk, sbk=sbk: e.tensor_copy(
                                out=tot[:, :, sbk], in_=psum[1][:, kk * H:(kk + 1) * H]),
                                reads=[ps_r[1]], writes=[r_cs])
                    for h in range(H):
                        P.op("dve", lambda e, h=h: e.tensor_tensor_scan(
                            out=pre[:, h, :], data0=onesrow[:], data1=tot[:, h, :], initial=0.0,
                            op0=ALU.mult, op1=ALU.add), reads=[r_cs], writes=[r_cs])
                    P.op("dve", lambda e: e.tensor_tensor(out=pre[:], in0=pre[:], in1=tot[:], op=ALU.subtract),
                         reads=[r_cs], writes=[r_cs])
                    for kbi in range(NK):
                        rp, lp = divmod(kbi, NBLK)
                        sbk = seq_block(rp, lp)
                        P.op("dve", lambda e, kbi=kbi, sbk=sbk: e.tensor_tensor(
                            out=W[:, kbi, :], in0=W[:, kbi, :], in1=pre[:, :, sbk], op=ALU.add),
                            reads=[r_cs], writes=[r_cs])
                    Wr = W[:].rearrange("p (r l) h -> p r l h", r=4)
                    P.op("dve", lambda e: e.tensor_scalar(out=ownc[:], in0=Wr[:, 0], scalar1=sel_sb[:, 0:1],
                                                          scalar2=None, op0=ALU.mult),
                         reads=[r_cs, r_const], writes=[r_cs])
                    for rp in range(1, 4):
                        P.op("dve", lambda e, rp=rp: e.scalar_tensor_tensor(
                            out=ownc[:], in0=Wr[:, rp], scalar=sel_sb[:, rp:rp + 1], in1=ownc[:],
                            op0=ALU.mult, op1=ALU.add), reads=[r_cs, r_const], writes=[r_cs])
                    P.op("dve", lambda e: e.tensor_scalar(out=ownc[:], in0=ownc[:], scalar1=-8.0, scalar2=None,
                                                          op0=ALU.mult), reads=[r_cs], writes=[r_cs])
                    for j in range(3):
                        P.op("dve", lambda e, j=j: e.tensor_copy(out=c3[:, :, :, j], in_=ownc[:]),
                             reads=[r_cs], writes=[r_cs])
                        if j < 2:
                            P.op("dve", lambda e, j=j: e.tensor_tensor(out=ownc[:], in0=ownc[:], in1=c3[:, :, :, j],
                                                                      op=ALU.subtract), reads=[r_cs], writes=[r_cs])
                    for l in range(NBLK):
                        tbk = 2 + (l % 2)
                        pT = psum[tbk][:].bitcast(BF16)
                        P.op("pe", lambda e, l=l, pT=pT: e.transpose(
                            out=pT[0:48, 0:128], in_=c3[:, l].rearrange("p h j -> p (h j)"), identity=identb[:]),
                            reads=[r_cs, r_const], writes=[ps_r[tbk]])
                        P.op("act", lambda e, l=l, pT=pT: e.activation(out=augT[:, l, :], in_=pT[0:48, 0:128],
                                                                      func=ACT.Copy),
                             reads=[ps_r[tbk]], writes=[r_aug])
                    for h in range(H):
                        P.dma("sp", kv_src[li][h][64:67, :], augT[3 * h:3 * h + 3, :, :].rearrange("p l t -> p (l t)"),
                              "staug", reads=[r_aug], writes=[r_kvs[h]], merge=True)
                    if stop_point("cum", P):
                        return
                    for h in range(H):
                        P.collective(cfg.groups, kv_src[li][h].ap().opt(), kv_dst[li][h].ap().opt(), f"cc_kv{h % 4}",
                                     reads=[r_kvs[h]], writes=[r_kvd[h]])
                    P.barrier(skip_cc=True)
                if stop_point("coll", P):
                    return
                o_all = sb("o_all", [128, NBLK, D], BF16, stack=sl)
                r_o = Res("o_all")
                attention(li, 64, 70, 0.125, o_all, r_o)
                if stop_point("attn", P):
                    return
                out_proj(li, fox_wout_d, o_all, r_o)
            if stop_point("oproj", P):
                return
            moe(li)


        def mla_layer(li):
            DK = 96
            scale = 96.0 ** -0.5
            load_mod(li)
            with contextlib.ExitStack() as sl:
                with contextlib.ExitStack() as s1:
                    win = sb("mwin", [128, 8, 672], BF16, stack=s1)
                    wuq = sb("wuq", [128, 3, 1536], BF16, stack=s1)
                    wukv = sb("wukv", [128, 2, 2048], BF16, stack=s1)
                    r_w = Res("mla_w")
                    r_wq = Res("mla_wuq")
                    r_wkv = Res("mla_wukv")
                    gl = sb("gl", [128, 640], stack=s1)
                    gqk = sb("gqk", [128, 2, 96], stack=s1)
                    invf = sb("invf_sb", [128, 16], stack=s1)
                    posi = sb("posi", [128, NBLK], I32, stack=s1)
                    posf = sb("posf", [128, NBLK], stack=s1)
                    ang = sb("ang", [128, 2, NBLK, 16], stack=s1)
                    kq = sb("kq", [128, 2, NBLK, 16], stack=s1)
                    kqi = sb("kqi", [128, 2, NBLK, 16], I32, stack=s1)
                    trig = sb("trig", [128, 2, NBLK, 16], stack=s1)
                    r_g = Res("mla_g")
                    r_trig = Res("trig")
                    ss = sb("ss1", [128, NBLK], stack=s1)
                    rstd = sb("rstd1", [128, NBLK], stack=s1)
                    r_ss = Res("ss1")
                    h32 = [sb(f"h32m_{i}", [128, D], stack=s1) for i in range(2)]
                    r_h32 = [Res(f"h32m_{i}") for i in range(2)]
                    hT = [sb(f"hTm_{i}", [128, 8, 128], BF16, stack=s1) for i in range(2)]
                    r_hT = [Res(f"hTm_{i}") for i in range(2)]
                    pj32 = sb("pj32", [128, 672], stack=s1)
                    r_pj = Res("pj32")
                    ssl = sb("ssl", [128, 4], stack=s1)
                    r_ssl = Res("ssl")
                    latn = sb("latn", [128, 640], BF16, stack=s1)
                    r_latn = Res("latn")
                    latT = sb("latT", [128, 5, 128], BF16, stack=s1)
                    r_latT = Res("latT")
                    QK32s = [sb(f"QK32_{i}", [128, 2, H, 96], stack=s1) for i in range(2)]
                    r_QKs = [Res(f"QK32_{i}") for i in range(2)]
                    SQ = sb("SQ", [128, 2, H, 96], stack=s1)
                    r_SQ = Res("SQ")
                    SQf = SQ[:].rearrange("p w h d -> p (w h d)")
                    tmpn, r_tmpn = SQf[:, 0:D], r_SQ
                    junk, r_junk = SQf[:, D:2 * D], r_SQ
                    junkA = sb("junkA", [128, 384], stack=s1)
                    r_junkA = Res("junkA")
                    rt = [SQf[:, i * 512:(i + 1) * 512].rearrange("p (a c) -> p a c", c=16) for i in range(4)]
                    ssqk = sb("ssqk", [128, 2, H], stack=s1)
                    rsqk = sb("rsqk", [128, 2, H], stack=s1)
                    r_ssqk = Res("ssqk")
                    r_rt = r_SQ
                    QKB = sb("QKB", [128, 2, H, 96], BF16, stack=s1)
                    r_QKB = Res("QKB")
                    stT0 = sb("stT0", [96, 2, H, 128], BF16, stack=s1)
                    stT = [stT0, stT0]
                    r_stT0 = Res("stT0")
                    r_stT = [r_stT0, r_stT0]
                    vsts = [sb(f"vst1_{i}", [128, H, 65], BF16, stack=s1) for i in range(2)]
                    r_vsts = [Res(f"vst1_{i}") for i in range(2)]

                    P.dma("pool", win[:], mla_win_d.ap().rearrange("(kc p) n -> p kc n", p=128), writes=[r_w])
                    P.dma("pool", wuq[:], mla_wuq_d.ap().rearrange("(kc p) n -> p kc n", p=128), writes=[r_wq])
                    P.dma("pool", wukv[:], mla_wukv_d.ap().rearrange("(kc p) n -> p kc n", p=128), writes=[r_wkv])
                    P.dma("sp", gl[:, 0:384], mla_qln_d[0:1, :].partition_broadcast(128), writes=[r_g])
                    P.dma("sp", gl[:, 384:640], mla_kvln_d[0:1, :].partition_broadcast(128), writes=[r_g])
                    P.dma("sp", gqk[:, 0, :], mla_qn_d[0:1, :].partition_broadcast(128), writes=[r_g])
                    P.dma("sp", gqk[:, 1, :], mla_kn_d[0:1, :].partition_broadcast(128), writes=[r_g])
                    P.dma("sp", invf[:], invf_d[:, :], writes=[r_g])
                    P.dma("sp", posi[:], pos_d[:, :], writes=[r_g])
                    for i_ in range(2):
                        P.op("pool", lambda e, i_=i_: e.memset(vsts[i_][:, :, 64:65], 1.0), writes=[r_vsts[i_]])
                    TWO_PI = 6.283185307179586
                    C1 = 6.28125
                    C2 = TWO_PI - C1

                    def dt_(fn, reads=(), writes=()):
                        P.op("dve", fn, reads=[r_g, r_trig] + list(reads), writes=[r_trig] + list(writes))

                    dt_(lambda e: e.tensor_copy(out=posf[:], in_=posi[:]))
                    dt_(lambda e: e.tensor_tensor(out=ang[:, 0], in0=posf[:].unsqueeze(2).to_broadcast([128, NBLK, 16]),
                                                  in1=invf[:].unsqueeze(1).to_broadcast([128, NBLK, 16]), op=ALU.mult))
                    dt_(lambda e: e.tensor_scalar(out=ang[:, 1], in0=ang[:, 0], scalar1=TWO_PI / 4, scalar2=None,
                                                  op0=ALU.add))
                    dt_(lambda e: e.tensor_scalar(out=kq[:], in0=ang[:], scalar1=1.0 / TWO_PI, scalar2=None,
                                                  op0=ALU.mult))
                    dt_(lambda e: e.tensor_copy(out=kqi[:], in_=kq[:]))
                    dt_(lambda e: e.tensor_copy(out=kq[:], in_=kqi[:]))
                    dt_(lambda e: e.scalar_tensor_tensor(out=ang[:], in0=kq[:], scalar=-C1, in1=ang[:],
                                                         op0=ALU.mult, op1=ALU.add))
                    dt_(lambda e: e.scalar_tensor_tensor(out=ang[:], in0=kq[:], scalar=-C2, in1=ang[:],
                                                         op0=ALU.mult, op1=ALU.add))
                    dt_(lambda e: e.tensor_scalar(out=ang[:], in0=ang[:], scalar1=3.1415925, scalar2=-3.1415925,
                                                  op0=ALU.min, op1=ALU.max))
                    P.op("act", lambda e: e.activation(out=trig[:], in_=ang[:], func=ACT.Sin),
                         reads=[r_trig], writes=[r_trig])

                    norm_mod(li, 1, mixn_d, None, tmpn, r_tmpn)
                    sumsq_blocks(ss, r_ss, junk, r_junk)
                    rstd_all(ss[:], rstd[:], r_ss, D)
                    bank_ctr = {"i": 0}

                    def nb():
                        b_ = 2 + (bank_ctr["i"] % 6)
                        bank_ctr["i"] += 1
                        return b_

                    def blockA(l):
                        b = l % 2
                        QK32, r_QK, vst, r_vst = QK32s[b], r_QKs[b], vsts[b], r_vsts[b]
                        yield from make_hT(l, 0, 1, rstd, r_ss, h32[b], r_h32[b], hT[b], r_hT[b], (0, 1))
                        for (c0, c1) in ((0, 512), (512, 672)):
                            bank = nb()
                            for kc in range(8):
                                P.op("pe", lambda e, kc=kc, c0=c0, c1=c1, bank=bank, b=b: e.matmul(
                                    psum[bank][:, 0:c1 - c0], lhsT=hT[b][:, kc, :], rhs=win[:, kc, c0:c1],
                                    start=(kc == 0), stop=(kc == 7)),
                                    reads=[r_hT[b], r_w], writes=[ps_r[bank]], signal=(kc == 7))
                            P.op("act", lambda e, c0=c0, c1=c1, bank=bank: e.activation(
                                out=pj32[:, c0:c1], in_=psum[bank][:, 0:c1 - c0], func=ACT.Copy),
                                reads=[ps_r[bank]], writes=[r_pj])
                            yield
                        P.op("act", lambda e: e.activation(out=junkA[:, 0:384], in_=pj32[:, 0:384], func=ACT.Square,
                                                           accum_out=ssl[:, 0:1]), reads=[r_pj], writes=[r_junkA, r_ssl])
                        P.op("act", lambda e: e.activation(out=junkA[:, 0:256], in_=pj32[:, 384:640], func=ACT.Square,
                                                           accum_out=ssl[:, 1:2]), reads=[r_pj], writes=[r_junkA, r_ssl])
                        P.op("act", lambda e: e.activation(out=junkA[:, 0:32], in_=pj32[:, 640:672], func=ACT.Square,
                                                           accum_out=ssl[:, 2:3]), reads=[r_pj], writes=[r_junkA, r_ssl])
                        P.op("act", lambda e: e.activation(out=ssl[:, 0:1], in_=ssl[:, 0:1], func=ACT.Sqrt,
                                                           scale=1.0 / 384, bias=EPS), reads=[r_ssl], writes=[r_ssl])
                        P.op("act", lambda e: e.activation(out=ssl[:, 1:2], in_=ssl[:, 1:2], func=ACT.Sqrt,
                                                           scale=1.0 / 256, bias=EPS), reads=[r_ssl], writes=[r_ssl])
                        P.op("dve", lambda e: e.reciprocal(out=ssl[:, 0:2], in_=ssl[:, 0:2]), reads=[r_ssl], writes=[r_ssl])
                        yield
                        P.op("dve", lambda e: e.scalar_tensor_tensor(out=latn[:, 0:384], in0=pj32[:, 0:384],
                                                                    scalar=ssl[:, 0:1], in1=gl[:, 0:384],
                                                                    op0=ALU.mult, op1=ALU.mult),
                             reads=[r_pj, r_ssl, r_g], writes=[r_latn])
                        P.op("dve", lambda e: e.scalar_tensor_tensor(out=latn[:, 384:640], in0=pj32[:, 384:640],
                                                                    scalar=ssl[:, 1:2], in1=gl[:, 384:640],
                                                                    op0=ALU.mult, op1=ALU.mult),
                             reads=[r_pj, r_ssl, r_g], writes=[r_latn])
                        yield
                        bank = nb()
                        pT = psum[bank][:].bitcast(BF16)
                        for k5 in range(5):
                            P.op("pe", lambda e, k5=k5, pT=pT: e.transpose(
                                out=pT[:, k5 * 128:(k5 + 1) * 128], in_=latn[:, k5 * 128:(k5 + 1) * 128],
                                identity=identb[:]), reads=[r_latn, r_const], writes=[ps_r[bank]], signal=(k5 == 4))
                        P.op("act", lambda e, pT=pT: e.activation(
                            out=latT[:], in_=pT[:, 0:640].rearrange("p (k t) -> p k t", k=5), func=ACT.Copy),
                            reads=[ps_r[bank]], writes=[r_latT])
                        yield
                        qflat = QK32[:, 0].rearrange("p h d -> p (h d)")
                        for ci in range(3):
                            bank = nb()
                            for kc in range(3):
                                P.op("pe", lambda e, kc=kc, ci=ci, bank=bank: e.matmul(
                                    psum[bank][:, :], lhsT=latT[:, kc, :], rhs=wuq[:, kc, ci * 512:(ci + 1) * 512],
                                    start=(kc == 0), stop=(kc == 2)),
                                    reads=[r_latT, r_wq], writes=[ps_r[bank]], signal=(kc == 2))
                            P.op("act", lambda e, ci=ci, bank=bank: e.activation(
                                out=qflat[:, ci * 512:(ci + 1) * 512], in_=psum[bank][:, :], func=ACT.Copy),
                                reads=[ps_r[bank]], writes=[r_QK])
                            yield
                        for ci in range(4):
                            bank = nb()
                            for kc in range(2):
                                P.op("pe", lambda e, kc=kc, ci=ci, bank=bank: e.matmul(
                                    psum[bank][:, :], lhsT=latT[:, 3 + kc, :], rhs=wukv[:, kc, ci * 512:(ci + 1) * 512],
                                    start=(kc == 0), stop=(kc == 1)),
                                    reads=[r_latT, r_wkv], writes=[ps_r[bank]], signal=(kc == 1))
                            pv = psum[bank][:, :].rearrange("p (h x) -> p h x", x=128)
                            P.op("act", lambda e, ci=ci, pv=pv: e.activation(
                                out=QK32[:, 1, ci * 4:(ci + 1) * 4, 0:64], in_=pv[:, :, 0:64], func=ACT.Copy),
                                reads=[ps_r[bank]], writes=[r_QK])
                            P.op("act", lambda e, ci=ci, pv=pv: e.activation(
                                out=vst[:, ci * 4:(ci + 1) * 4, 0:64], in_=pv[:, :, 64:128], func=ACT.Copy),
                                reads=[ps_r[bank]], writes=[r_vst])
                            yield
                        P.op("dve", lambda e: e.tensor_copy(
                            out=QK32[:, 1, :, 64:96], in_=pj32[:, 640:672].unsqueeze(1).to_broadcast([128, H, 32])),
                            reads=[r_pj], writes=[r_QK])
                        yield

                    def blockB(l):
                        b = l % 2
                        QK32, r_QK, vst, r_vst = QK32s[b], r_QKs[b], vsts[b], r_vsts[b]
                        P.op("act", lambda e: e.activation(out=SQ[:], in_=QK32[:], func=ACT.Square),
                             reads=[r_QK], writes=[r_SQ])
                        P.op("dve", lambda e: e.tensor_reduce(out=ssqk[:], in_=SQ[:], axis=AX.X, op=ALU.add),
                             reads=[r_SQ], writes=[r_ssqk])
                        yield
                        P.op("act", lambda e: e.activation(out=rsqk[:], in_=ssqk[:], func=ACT.Sqrt, scale=1.0 / 96,
                                                           bias=EPS), reads=[r_ssqk], writes=[r_ssqk])
                        P.op("dve", lambda e: e.reciprocal(out=rsqk[:], in_=rsqk[:]), reads=[r_ssqk], writes=[r_ssqk])
                        yield
                        P.op("dve", lambda e: e.tensor_tensor(
                            out=QK32[:], in0=QK32[:], in1=rsqk[:].unsqueeze(3).to_broadcast([128, 2, H, 96]),
                            op=ALU.mult), reads=[r_QK, r_ssqk], writes=[r_QK])
                        yield
                        P.op("pool", lambda e: e.tensor_tensor(
                            out=QK32[:, :, :, 64:96], in0=QK32[:, :, :, 64:96],
                            in1=gqk[:, :, 64:96].unsqueeze(2).to_broadcast([128, 2, H, 32]),
                            op=ALU.mult), reads=[r_QK, r_g], writes=[r_QK])
                        yield
                        QKv = QK32[:].rearrange("p w h d -> p (w h) d")
                        QBv = QKB[:].rearrange("p w h d -> p (w h) d")
                        x1 = QKv[:, :, 64:80]
                        x2 = QKv[:, :, 80:96]
                        sn = trig[:, 0, l, :].unsqueeze(1).to_broadcast([128, 2 * H, 16])
                        cs = trig[:, 1, l, :].unsqueeze(1).to_broadcast([128, 2 * H, 16])
                        P.op("dve", lambda e: e.tensor_tensor(
                            out=QKB[:, :, :, 0:64], in0=QK32[:, :, :, 0:64],
                            in1=gqk[:, :, 0:64].unsqueeze(2).to_broadcast([128, 2, H, 64]), op=ALU.mult),
                             reads=[r_QK, r_g], writes=[r_QKB])
                        yield
                        P.op("dve", lambda e, x1=x1, cs=cs: e.tensor_tensor(out=rt[0][:], in0=x1, in1=cs, op=ALU.mult),
                             reads=[r_QK, r_trig], writes=[r_rt])
                        P.op("pool", lambda e, x2=x2, sn=sn: e.tensor_tensor(out=rt[1][:], in0=x2, in1=sn, op=ALU.mult),
                             reads=[r_QK, r_trig], writes=[r_rt])
                        yield
                        P.op("dve", lambda e, x1=x1, sn=sn: e.tensor_tensor(out=rt[2][:], in0=x1, in1=sn, op=ALU.mult),
                             reads=[r_QK, r_trig], writes=[r_rt])
                        P.op("pool", lambda e, x2=x2, cs=cs: e.tensor_tensor(out=rt[3][:], in0=x2, in1=cs, op=ALU.mult),
                             reads=[r_QK, r_trig], writes=[r_rt])
                        yield
                        P.op("dve", lambda e: e.tensor_tensor(out=QBv[:, :, 64:80], in0=rt[0][:], in1=rt[1][:],
                                                              op=ALU.subtract), reads=[r_rt], writes=[r_QKB])
                        P.op("dve", lambda e: e.tensor_tensor(out=QBv[:, :, 80:96], in0=rt[2][:], in1=rt[3][:],
                                                              op=ALU.add), reads=[r_rt], writes=[r_QKB])
                        yield
                        sT = stT[b]
                        sTv = sT[:].rearrange("p w h t -> p (w h) t")
                        for g8 in range(4):
                            bank = nb()
                            pT = psum[bank][:].bitcast(BF16)
                            for h8 in range(8):
                                wh = g8 * 8 + h8
                                P.op("pe", lambda e, wh=wh, h8=h8, pT=pT: e.transpose(
                                    out=pT[0:96, h8 * 128:(h8 + 1) * 128], in_=QBv[:, wh, :], identity=identb[:]),
                                    reads=[r_QKB, r_const], writes=[ps_r[bank]], signal=(h8 == 7))
                            P.op("act", lambda e, g8=g8, pT=pT, sTv=sTv: e.activation(
                                out=sTv[:, g8 * 8:(g8 + 1) * 8, :],
                                in_=pT[0:96, 0:1024].rearrange("p (h t) -> p h t", h=8), func=ACT.Copy),
                                reads=[ps_r[bank]], writes=[r_stT[b]])
                            yield
                        P.dma("sp", dap(qT_d[li], l * 128, [[T, 96], [DKMAX * T, H], [1, 128]]), sT[:, 0],
                              reads=[r_stT[b]], writes=[r_qTd], merge=True)
                        for h in range(H):
                            P.dma("sp", kv_src[li][h][0:96, l * 128:(l + 1) * 128], sT[:, 1, h, :],
                                  reads=[r_stT[b]], writes=[r_kvs[h]], merge=True)
                            P.dma("sp", dap(kv_src[li][h], 96 * T + l * 65, [[NBLK * 65, 128], [1, 65]]),
                                  vst[:, h, :], reads=[r_vst], writes=[r_kvs[h]], merge=True)
                        yield

                    def run_interleaved(gens):
                        gens = list(gens)
                        while gens:
                            for g_ in list(gens):
                                try:
                                    next(g_)
                                except StopIteration:
                                    gens.remove(g_)

                    run_interleaved([blockA(0)])
                    for l in range(NBLK):
                        gl_ = [blockA(l + 1)] if l + 1 < NBLK else []
                        run_interleaved(gl_ + [blockB(l)])
                    for h in range(H):
                        P.collective(cfg.groups, kv_src[li][h].ap().opt(), kv_dst[li][h].ap().opt(),
                                     reads=[r_kvs[h]], writes=[r_kvd[h]])
                    P.barrier(skip_cc=True)
                if stop_point("coll1", P):
                    return
                o_all = sb("o_all1", [128, NBLK, D], BF16, stack=sl)
                r_o = Res("o_all1")
                attention(li, 96, 96, scale, o_all, r_o)
                if stop_point("attn1", P):
                    return
                out_proj(li, mla_wout_d, o_all, r_o)
            if stop_point("oproj1", P):
                return
            moe(li)

        if not stop_point("p0", P):
            fox_layer(0)
            if depth > 1 and not P.__dict__.get("stopped", False):
                for r_ in [r_qTd] + r_kvs + r_kvd:
                    r_.writers = {}
                    r_.readers = {}
                if not stop_point("l0", P):
                    mla_layer(1)
        for l in range(NBLK):
            P.dma("sp", y_d[l * 128:(l + 1) * 128, :], x_sb[:, l, :], f"sty{l % 4}", reads=[x_r[l]])
        P.finish()
        print("instr counts", P.ninstr, "sems", len(P.sems))
    return nc


def make_in_maps(cfg, inp):
    f32 = np.float32
    B, S, T, NBLK = cfg.B, cfg.S, cfg.T, cfg.NBLK
    depth, E = cfg.depth, cfg.E
    maps = []
    invf = np.broadcast_to((10000.0 ** (-np.arange(16, dtype=np.float32) / np.float32(16))).astype(f32)[None], (128, 16))
    kidx = np.zeros((128, 8), f32)
    for rp in range(4):
        for tp in range(2):
            kidx[:, rp * 2 + tp] = seq_block(rp, tp) * 128 + np.arange(128)
    padw = np.zeros(inp["moe_w_grp"].shape[:2] + (cfg.NRP - cfg.NR,), f32)
    padb = np.zeros(inp["moe_b_grp"].shape[:1] + (cfg.NRP - cfg.NR,), f32)
    wr = np.concatenate([inp["moe_w_grp"], inp["moe_w_rt"], padw], axis=-1).astype(f32)
    br = np.concatenate([inp["moe_b_grp"], inp["moe_b_rt"], padb], axis=-1).astype(f32)
    for b in range(B):
        for r in range(4):
            blocks = [seq_block(r, l) for l in range(NBLK)]
            xs = np.concatenate([inp["x"][b, sb_ * 128:(sb_ + 1) * 128] for sb_ in blocks], axis=0)
            ps = np.stack([inp["positions"][b, sb_ * 128:(sb_ + 1) * 128] for sb_ in blocks], axis=1)
            qidx = np.concatenate([seq_block(r, t) * 128 + np.arange(128) for t in range(2)])[None].astype(f32)
            sel = np.zeros((128, 4), f32)
            sel[:, r] = 1.0
            m = {
                "x": np.ascontiguousarray(xs, dtype=f32),
                "c": np.ascontiguousarray(inp["c"][b:b + 1], dtype=f32),
                "pos": np.ascontiguousarray(ps, dtype=np.int32),
                "qidx": qidx, "kidx": kidx, "sel": sel, "invf": invf,
                "w_ada": np.ascontiguousarray(inp["w_ada"][:, :, r * cfg.ADAW:(r + 1) * cfg.ADAW], dtype=f32),
                "b_ada": np.ascontiguousarray(inp["b_ada"][:, r * cfg.ADAW:(r + 1) * cfg.ADAW], dtype=f32),
                "mix_norm": inp["mix_norm"], "ffn_norm": inp["ffn_norm"],
                "fox_w_in": inp["fox_w_in"][0], "fox_b_f": inp["fox_b_f"][0:1],
                "fox_q_norm": inp["fox_q_norm"][0:1], "fox_k_norm": inp["fox_k_norm"][0:1],
                "fox_w_out": inp["fox_w_out"][0],
                "moe_w_r": wr, "moe_b_r": br,
                "moe_w_gate": inp["moe_w_gate"].reshape(depth * E, D, cfg.F),
                "moe_w_up": inp["moe_w_up"].reshape(depth * E, D, cfg.F),
                "moe_w_down": inp["moe_w_down"].reshape(depth * E, cfg.F, D),
            }
            if depth > 1:
                m.update({
                    "mla_w_in": inp["mla_w_in"][0], "mla_q_lat_norm": inp["mla_q_lat_norm"][0:1],
                    "mla_kv_lat_norm": inp["mla_kv_lat_norm"][0:1], "mla_w_uq": inp["mla_w_uq"][0],
                    "mla_w_ukv": inp["mla_w_ukv"][0], "mla_q_norm": inp["mla_q_norm"][0:1],
                    "mla_k_norm": inp["mla_k_norm"][0:1], "mla_w_out": inp["mla_w_out"][0],
                })
            maps.append({k: np.ascontiguousarray(v) for k, v in m.items()})
    return maps


def assemble(cfg, results):
    out = np.zeros((cfg.B, cfg.S, D), np.float32)
    for b in range(cfg.B):
        for r in range(4):
            y = results[b * 4 + r]["y"]
            for l in range(cfg.NBLK):
                sb_ = seq_block(r, l)
                out[b, sb_ * 128:(sb_ + 1) * 128] = y[l * 128:(l + 1) * 128]
    return out


def run(cfg, inp, trace=False):
    nc = build_program(cfg)
    maps = make_in_maps(cfg, inp)
    res = run_bass_kernel_spmd(nc, maps, core_ids=list(range(cfg.NCORE)), trace=trace)
    return assemble(cfg, res.results), res


def kernel(**inputs):
    inp = {k: np.asarray(v) for k, v in inputs.items()}
    cfg = Cfg(B=2, S=8192, depth=2)
    out, _ = run(cfg, inp)
    return out
```

```python
import contextlib
import numpy as np
import ml_dtypes
import concourse.bass as bass
import concourse.mybir as mybir
from concourse.bass_utils import run_bass_kernel_spmd

F32 = mybir.dt.float32
BF16 = mybir.dt.bfloat16
I32 = mybir.dt.int32
ACT = mybir.ActivationFunctionType
ALU = mybir.AluOpType
AX = mybir.AxisListType

import os
ENGS = ("pe", "act", "dve", "pool", "sp")
STOP = os.environ.get("MK_STOP", "")


class StopBuild(Exception):
    pass


def stop_point(name, P):
    if STOP == name:
        P.barrier()
        P.stopped = True
        print("build stopped at", name)
    return getattr(P, "stopped", False)
D = 1024
H = 16
EPS = 1e-6
NEG = -30000.0


class Res:
    def __init__(self, name, psum=False):
        self.name = name
        self.psum = psum
        self.writers = {}
        self.readers = {}
        self.pend_w = None
        self.pend_r = set()


RING = {"sp": 24, "pool": 16, "act": 4}


class Prog:
    def __init__(self, nc, stack):
        self.nc = nc
        self.stack = stack
        self.ops = {e: [] for e in ENGS}
        self.sems = {}
        self.cnt = {e: 0 for e in ENGS}
        self.waited = {e: {} for e in ENGS}
        self.dmaval = {}
        self.pend = {e: ([], []) for e in ENGS}
        self.ninstr = {e: 0 for e in ENGS}
        self.ring_i = {e: 0 for e in RING}
        self.cc_i = 0
        for e in ENGS:
            self.sem("E_" + e)
        for e, n in RING.items():
            for i in range(n):
                self.sem(f"D_{e}{i}")
        for i in range(16):
            self.sem(f"C_{i}")

    def sem(self, name):
        if name not in self.sems:
            self.sems[name] = self.stack.enter_context(self.nc.semaphore(name))
            self.dmaval[name] = 0
        return name

    def _need(self, eng, reads, writes, extra, merge=False):
        need = {}

        def add(s, v):
            if need.get(s, 0) < v:
                need[s] = v

        own = "E_" + eng
        for r in reads:
            assert r.pend_w is None or r.pend_w == eng, f"read of {r.name}: unsignaled write by {r.pend_w}"
            for s, v in r.writers.items():
                add(s, v)
            if r.psum:
                for s, v in r.readers.items():
                    if s != own:
                        add(s, v)
        if not merge:
            for w in writes:
                assert w.pend_w is None or w.pend_w == eng, f"write of {w.name}: unsignaled write by {w.pend_w}"
                assert not (w.pend_r - {eng}), f"write of {w.name}: unsignaled reads by {w.pend_r}"
                for s, v in w.writers.items():
                    add(s, v)
                for s, v in w.readers.items():
                    add(s, v)
        for (s, v) in extra:
            add(s, v)
        if eng == "pe":
            need.pop(own, None)
        out = []
        for s, v in need.items():
            if self.waited[eng].get(s, 0) < v:
                self.waited[eng][s] = v
                out.append((s, v))
        return out

    def _emit_waits(self, eng, waits):
        for s, v in waits:
            h = self.sems[s]
            self.ops[eng].append(lambda e, h=h, v=v: e.wait_ge(h, v))

    def op(self, eng, fn, reads=(), writes=(), signal=True, extra=()):
        waits = self._need(eng, reads, writes, extra)
        self._emit_waits(eng, waits)
        self.ninstr[eng] += 1
        pr, pw = self.pend[eng]
        if not signal:
            self.ops[eng].append(lambda e, fn=fn: fn(e))
            for r in reads:
                pr.append(r)
                r.pend_r.add(eng)
            for w in writes:
                pw.append(w)
                w.pend_w = eng
            return None
        own = "E_" + eng
        self.cnt[eng] += 1
        tok = (own, self.cnt[eng])
        h = self.sems[own]
        self.ops[eng].append(lambda e, fn=fn, h=h: fn(e).then_inc(h, 1))
        for r in list(reads) + pr:
            r.readers[own] = tok[1]
            r.pend_r.discard(eng)
        for w in list(writes) + pw:
            w.writers = {own: tok[1]}
            w.readers = {}
            w.pend_w = None
        self.pend[eng] = ([], [])
        return tok

    def dma(self, eng, out, in_, sem=None, reads=(), writes=(), extra=(), merge=False, **kw):
        i = self.ring_i[eng]
        self.ring_i[eng] = (i + 1) % RING[eng]
        sem = f"D_{eng}{i}"
        extra = list(extra)
        if self.dmaval[sem] > 0:
            extra.append((sem, self.dmaval[sem]))
        waits = self._need(eng, reads, writes, extra, merge=merge)
        self._emit_waits(eng, waits)
        self.dmaval[sem] += 16
        tok = (sem, self.dmaval[sem])
        h = self.sems[sem]
        self.ninstr[eng] += 1
        self.ops[eng].append(
            lambda e, out=out, in_=in_, h=h, kw=kw: e.dma_start(out=out, in_=in_, **kw).then_inc(h, 16))
        for r in reads:
            r.readers[sem] = tok[1]
        for w in writes:
            if merge:
                w.writers[sem] = tok[1]
            else:
                w.writers = {sem: tok[1]}
                w.readers = {}
        return tok

    def collective(self, groups, in_ap, out_ap, sem=None, reads=(), writes=()):
        eng = "pool"
        sem = f"C_{self.cc_i % 16}"
        self.cc_i += 1
        extra = [(sem, self.dmaval[sem])] if self.dmaval[sem] > 0 else []
        waits = self._need(eng, reads, writes, extra)
        self._emit_waits(eng, waits)
        self.dmaval[sem] += 1
        tok = (sem, self.dmaval[sem])
        h = self.sems[sem]
        self.ninstr[eng] += 1
        self.ops[eng].append(
            lambda e: e.collective_compute("AllGather", ALU.bypass, replica_groups=groups,
                                           ins=[in_ap], outs=[out_ap]).then_inc(h))
        for r in reads:
            r.readers[sem] = tok[1]
        for w in writes:
            w.writers = {sem: tok[1]}
            w.readers = {}
        return tok

    def wait_all(self, eng, skip_cc=False):
        waits = []
        for s in self.sems:
            if skip_cc and s.startswith("C_"):
                continue
            v = self.cnt[s[2:]] if s.startswith("E_") else self.dmaval[s]
            if s == "E_" + eng:
                continue
            if v > 0 and self.waited[eng].get(s, 0) < v:
                self.waited[eng][s] = v
                waits.append((s, v))
        self._emit_waits(eng, waits)

    def barrier(self, skip_cc=False):
        for e in ENGS:
            assert not self.pend[e][0] and not self.pend[e][1], f"pending unsignaled ops on {e}"
        for e in ENGS:
            self.wait_all(e, skip_cc=skip_cc)

    def finish(self):
        self.barrier()
        nc = self.nc
        ops = self.ops
        with nc.Block() as block:
            @block.tensor
            def _(e):
                for f in ops["pe"]:
                    f(e)

            @block.scalar
            def _(e):
                for f in ops["act"]:
                    f(e)

            @block.vector
            def _(e):
                for f in ops["dve"]:
                    f(e)

            @block.gpsimd
            def _(e):
                for f in ops["pool"]:
                    f(e)

            @block.sync
            def _(e):
                for f in ops["sp"]:
                    f(e)


class Cfg:
    def __init__(self, B=2, S=8192, depth=2, G=4, EPG=4, F=256):
        self.B, self.S, self.depth = B, S, depth
        self.G, self.EPG, self.E, self.F = G, EPG, G * EPG, F
        self.RPG = 4
        self.NCORE = B * self.RPG
        self.T = S // self.RPG
        self.NBLK = self.T // 128
        self.NM = self.NBLK // 2
        assert self.NBLK % 2 == 0 and self.NM >= 1
        self.ADAW = 6 * D // self.RPG
        self.groups = [[b * 4 + r for r in range(4)] for b in range(B)]
        self.FOX_IN = 3 * D + H
        self.NR = G + self.E
        self.NRP = 32


def seq_block(r, l):
    m, t = divmod(l, 2)
    return 8 * m + (r if t == 0 else 7 - r)


def dap(t, offset, ap):
    return bass.AP(tensor=t, offset=offset, ap=ap)


def build_program(cfg):
    nc = bass.Bass("TRN2", target_bir_lowering=False)
    T, NBLK, NM, E, G, EPG, F = cfg.T, cfg.NBLK, cfg.NM, cfg.E, cfg.G, cfg.EPG, cfg.F
    S = cfg.S
    depth = cfg.depth
    NR = cfg.NR
    NRP = cfg.NRP
    ADAW = cfg.ADAW

    def din(name, shape, dt=F32):
        return nc.dram_tensor(name, list(shape), dt, kind="ExternalInput")

    x_d = din("x", [T, D])
    c_d = din("c", [1, D])
    pos_d = din("pos", [128, NBLK], I32)
    qidx_d = din("qidx", [1, 256])
    kidx_d = din("kidx", [128, 8])
    sel_d = din("sel", [128, 4])
    invf_d = din("invf", [128, 16])
    wada_d = din("w_ada", [depth, D, ADAW])
    bada_d = din("b_ada", [depth, ADAW])
    mixn_d = din("mix_norm", [depth, D])
    ffnn_d = din("ffn_norm", [depth, D])
    fox_win_d = din("fox_w_in", [D, cfg.FOX_IN])
    fox_bf_d = din("fox_b_f", [1, H])
    fox_qn_d = din("fox_q_norm", [1, 64])
    fox_kn_d = din("fox_k_norm", [1, 64])
    fox_wout_d = din("fox_w_out", [D, D])
    if depth > 1:
        mla_win_d = din("mla_w_in", [D, 672])
        mla_qln_d = din("mla_q_lat_norm", [1, 384])
        mla_kvln_d = din("mla_kv_lat_norm", [1, 256])
        mla_wuq_d = din("mla_w_uq", [384, 1536])
        mla_wukv_d = din("mla_w_ukv", [256, 2048])
        mla_qn_d = din("mla_q_norm", [1, 96])
        mla_kn_d = din("mla_k_norm", [1, 96])
        mla_wout_d = din("mla_w_out", [D, D])
    wrt_d = din("moe_w_r", [depth, D, NRP])
    brt_d = din("moe_b_r", [depth, NRP])
    wg_d = din("moe_w_gate", [depth * E, D, F])
    wu_d = din("moe_w_up", [depth * E, D, F])
    wd_d = din("moe_w_down", [depth * E, F, D])
    y_d = nc.dram_tensor("y", [T, D], F32, kind="ExternalOutput")

    DKMAX = 96
    mod_src = nc.dram_tensor("mod_src", [depth, ADAW], F32)
    mod_dst = nc.dram_tensor("mod_dst", [4 * depth, ADAW], F32)
    lf_src = nc.dram_tensor("lf_src", [128, NBLK * H], F32)
    lf_dst = nc.dram_tensor("lf_dst", [4 * 128, NBLK * H], F32)
    qT_d = [nc.dram_tensor(f"qT_{li}", [H, DKMAX, T], BF16) for li in range(depth)]
    ROWS = {0: 64 + 65, 1: 96 + 65}
    augk_d = nc.dram_tensor("augk", [H, 3, 4 * T], BF16)
    augq_d = nc.dram_tensor("augq", [H, 3, T], BF16)
    kv_src = [[nc.dram_tensor(f"kvs_{li}_{h}", [ROWS[li], T], BF16) for h in range(H)] for li in range(depth)]
    kv_dst = [[nc.dram_tensor(f"kvd_{li}_{h}", [4 * ROWS[li], T], BF16) for h in range(H)] for li in range(depth)]

    with contextlib.ExitStack() as st:
        P = Prog(nc, st)

        uniq = {"n": 0}

        def sb(name, shape, dt=F32, stack=st):
            uniq["n"] += 1
            return stack.enter_context(nc.sbuf_tensor(f"{name}_u{uniq['n']}", list(shape), dt))

        x_sb = sb("x_sb", [128, NBLK, D])
        x_r = [Res(f"x{l}") for l in range(NBLK)]
        identf = sb("identf", [128, 128])
        identb = sb("identb", [128, 128], BF16)
        onesf = sb("onesf", [128, 128])
        trif = sb("trif", [128, 128])
        zerob = sb("zerob", [128, 256], BF16)
        maskb = sb("maskb", [128, 8, 256], BF16)
        sel_sb = sb("sel_sb", [128, 4])
        modbc = sb("modbc", [128, 6 * D])
        r_const = Res("const")
        r_mod = Res("modbc")
        r_moddst = Res("mod_dst")

        pbig = [st.enter_context(nc.psum_tensor(f"pp{i}", [128, 1024], F32)) for i in range(4)]
        psum = [pbig[i // 2][:, (i % 2) * 512:(i % 2 + 1) * 512] for i in range(8)]
        ps_r = [Res(f"ps{i}", psum=True) for i in range(8)]

        for l in range(NBLK):
            P.dma("sp", x_sb[:, l, :], x_d[l * 128:(l + 1) * 128, :], f"ldx{l % 4}", writes=[x_r[l]])
        P.op("pool", lambda e: e.memset(onesf[:], 1.0), writes=[r_const], signal=False)
        P.op("pool", lambda e: e.memset(identf[:], 1.0), writes=[r_const], signal=False)
        P.op("pool", lambda e: e.memset(trif[:], 1.0), writes=[r_const], signal=False)
        P.op("pool", lambda e: e.memset(zerob[:], 0.0), writes=[r_const])
        P.op("pool", lambda e: e.affine_select(out=identf[:], in_=identf[:], pattern=[[-1, 128]],
                                               compare_op=ALU.is_equal, fill=0.0, base=0, channel_multiplier=1),
             reads=[r_const], writes=[r_const])
        P.op("pool", lambda e: e.affine_select(out=trif[:], in_=trif[:], pattern=[[1, 128]],
                                               compare_op=ALU.is_ge, fill=0.0, base=0, channel_multiplier=-1),
             reads=[r_const], writes=[r_const])
        P.op("dve", lambda e: e.tensor_copy(out=identb[:], in_=identf[:]), reads=[r_const], writes=[r_const])
        P.dma("sp", sel_sb[:], sel_d[:, :], "ldc", writes=[r_const])
        with contextlib.ExitStack() as s0:
            qib = sb("qib", [128, 256], stack=s0)
            kix = sb("kix", [128, 8], stack=s0)
            mtmp = sb("mtmp", [128, 256], stack=s0)
            cT = sb("cT", [128, 8], stack=s0)
            cS = sb("cS", [128, 8], stack=s0)
            cbc = sb("cbc", [128, 8, 128], stack=s0)
            wadas = [sb(f"wada{i}", [128, 8, ADAW], stack=s0) for i in range(min(depth, 2))]
            badat = sb("badat", [1, ADAW], stack=s0)
            modrow = badat
            r_t = Res("p0tmp")
            r_wadas = [Res(f"wada{i}") for i in range(2)]
            r_row = Res("modrow")
            r_bad = Res("badat")
            r_modsrc = Res("mod_src")
            P.dma("sp", qib[:], qidx_d[0:1, :].partition_broadcast(128), "ldc", writes=[r_t])
            P.dma("sp", kix[:], kidx_d[:, :], "ldc", writes=[r_t])
            P.dma("sp", cT[:], c_d.ap().rearrange("o (kc p) -> p (o kc)", p=128), "ldc", writes=[r_t],
                  allow_slow_non_contiguous=True)
            for j in range(8):
                P.op("dve", lambda e, j=j: e.tensor_scalar(out=mtmp[:], in0=qib[:], scalar1=kix[:, j:j + 1],
                                                          scalar2=None, op0=ALU.is_ge),
                     reads=[r_t], writes=[r_t])
                P.op("dve", lambda e, j=j: e.tensor_scalar(out=maskb[:, j, :], in0=mtmp[:], scalar1=-1.0,
                                                          scalar2=-NEG, op0=ALU.add, op1=ALU.mult),
                     reads=[r_t], writes=[r_const])
            P.op("act", lambda e: e.activation(out=cS[:], in_=cT[:], func=ACT.Silu), reads=[r_t], writes=[r_t])
            for kc in range(8):
                P.op("dve", lambda e, kc=kc: e.tensor_scalar(out=cbc[:, kc, :], in0=onesf[:], scalar1=cS[:, kc:kc + 1],
                                                            scalar2=None, op0=ALU.mult),
                     reads=[r_t, r_const], writes=[r_t])
            for li in range(depth):
                P.dma("sp", wadas[li % 2][:], wada_d[li].rearrange("(kc p) n -> p kc n", p=128), "ldw",
                      writes=[r_wadas[li % 2]])
            for li in range(depth):
                wada, r_wada = wadas[li % 2], r_wadas[li % 2]
                P.dma("sp", badat[:], bada_d[li:li + 1, :], "ldw", writes=[r_bad])
                nch = (ADAW + 511) // 512
                for ch in range(nch):
                    c0, c1 = ch * 512, min(ADAW, (ch + 1) * 512)
                    bank = ch % 2
                    for kc in range(8):
                        P.op("pe", lambda e, kc=kc, c0=c0, c1=c1, bank=bank, wada=wada: e.matmul(
                            psum[bank][:, 0:c1 - c0], lhsT=cbc[:, kc, :], rhs=wada[:, kc, c0:c1],
                            start=(kc == 0), stop=(kc == 7)),
                            reads=[r_t, r_wada], writes=[ps_r[bank]], signal=(kc == 7))
                    P.op("dve", lambda e, c0=c0, c1=c1, bank=bank: e.tensor_tensor(
                        out=modrow[0:1, c0:c1], in0=psum[bank][0:1, 0:c1 - c0], in1=badat[0:1, c0:c1], op=ALU.add),
                        reads=[ps_r[bank], r_bad], writes=[r_bad])
                P.dma("sp", mod_src[li:li + 1, :], modrow[:], "stm", reads=[r_bad], writes=[r_modsrc], merge=(li > 0))
            P.collective(cfg.groups, mod_src.ap().opt(), mod_dst.ap().opt(), "cc_mod", reads=[r_modsrc],
                         writes=[r_moddst])
            P.barrier()

        def rstd_all(ss, rstd, r_ss, n):
            P.op("act", lambda e: e.activation(out=rstd, in_=ss, func=ACT.Sqrt, scale=1.0 / n, bias=EPS),
                 reads=[r_ss], writes=[r_ss])
            P.op("dve", lambda e: e.reciprocal(out=rstd, in_=rstd), reads=[r_ss], writes=[r_ss])

        def load_mod(li):
            P.dma("sp", modbc[:].rearrange("p (r n) -> p r n", r=4),
                  dap(mod_dst, li * ADAW, [[0, 128], [depth * ADAW, 4], [1, ADAW]]), "ldmod",
                  reads=[r_moddst], writes=[r_mod])

        def norm_mod(li, which, normvec_d, s1, tmpn, r_tmpn):
            off = 1 if which == 1 else 4
            P.dma("sp", tmpn[:], normvec_d[li:li + 1, :].partition_broadcast(128), "ldn", writes=[r_tmpn])
            P.op("dve", lambda e: e.scalar_tensor_tensor(out=modbc[:, off * D:(off + 1) * D],
                                                        in0=modbc[:, off * D:(off + 1) * D], scalar=1.0,
                                                        in1=tmpn[:], op0=ALU.add, op1=ALU.mult),
                 reads=[r_mod, r_tmpn], writes=[r_mod])

        def sumsq_blocks(ss, r_ss, junk, r_junk):
            for l in range(NBLK):
                P.op("act", lambda e, l=l: e.activation(out=junk[:], in_=x_sb[:, l, :], func=ACT.Square,
                                                       accum_out=ss[:, l:l + 1]),
                     reads=[x_r[l]], writes=[r_junk, r_ss])

        def make_hT(l, off_sh, off_A, rstd, r_ss, h32, r_h32, hT_out, r_hT, tb, hT32=None, r_hT32=None):
            P.op("dve", lambda e: e.scalar_tensor_tensor(out=h32[:], in0=x_sb[:, l, :], scalar=rstd[:, l:l + 1],
                                                        in1=modbc[:, off_A * D:(off_A + 1) * D],
                                                        op0=ALU.mult, op1=ALU.mult),
                 reads=[x_r[l], r_ss, r_mod], writes=[r_h32])
            P.op("pool", lambda e: e.tensor_tensor(out=h32[:], in0=h32[:], in1=modbc[:, off_sh * D:(off_sh + 1) * D],
                                                  op=ALU.add),
                 reads=[r_h32, r_mod], writes=[r_h32])
            yield
            for half in range(2):
                bank = tb[half]
                for k4 in range(4):
                    kc = half * 4 + k4
                    P.op("pe", lambda e, kc=kc, k4=k4, bank=bank: e.transpose(
                        out=psum[bank][:, k4 * 128:(k4 + 1) * 128], in_=h32[:, kc * 128:(kc + 1) * 128],
                        identity=identf[:]),
                        reads=[r_h32, r_const], writes=[ps_r[bank]], signal=(k4 == 3))
                P.op("act", lambda e, half=half, bank=bank: e.activation(
                    out=hT_out[:, half * 4:(half + 1) * 4, :], in_=psum[bank][:].rearrange("p (k t) -> p k t", k=4),
                    func=ACT.Copy),
                    reads=[ps_r[bank]], writes=[r_hT])
                if hT32 is not None:
                    P.op("dve", lambda e, half=half, bank=bank: e.tensor_copy(
                        out=hT32[:, half * 4:(half + 1) * 4, :],
                        in_=psum[bank][:].rearrange("p (k t) -> p k t", k=4)),
                        reads=[ps_r[bank]], writes=[r_hT32])
                yield

        def run_interleaved(gens):
            gens = list(gens)
            while gens:
                for g_ in list(gens):
                    try:
                        next(g_)
                    except StopIteration:
                        gens.remove(g_)

        def attention(li, dk, KA, scale, o_all, r_o):
            rows = ROWS[li]
            with contextlib.ExitStack() as sa:
                KT = [sb(f"KT{i}", [KA, 4 * T], BF16, stack=sa) for i in range(2)]
                VV = [sb(f"VV{i}", [128, 4 * NBLK, 65], BF16, stack=sa) for i in range(2)]
                QT = [sb(f"QT{i}", [KA, T], BF16, stack=sa) for i in range(2)]
                PT = [sb(f"PT{i}", [128, 1024], BF16, stack=sa) for i in range(3)]
                rec = sb("rec", [128, 2], stack=sa)
                r_KT = [Res(f"KT{i}") for i in range(2)]
                r_VV = [Res(f"VV{i}") for i in range(2)]
                r_QT = [Res(f"QT{i}") for i in range(2)]
                r_PT = [Res(f"PT{i}") for i in range(3)]
                r_rec = Res("rec")
                SBIG = [pbig[0], pbig[1], pbig[2]]
                r_S = [[ps_r[0], ps_r[1]], [ps_r[2], ps_r[3]], [ps_r[4], ps_r[5]]]
                OB = [6, 6]
                TB = [7, 7]
                osb = [sb(f"osb{i}", [65, 256], stack=sa) for i in range(2)]
                r_osb = [Res(f"osb{i}") for i in range(2)]
                pending = []
                fin_ctr = {"i": 0}
                if li == 0:
                    for i in range(2):
                        P.op("pool", lambda e, i=i: e.memset(KT[i][64:70, :], -1.0), writes=[r_KT[i]])
                        P.op("pool", lambda e, i=i: e.memset(QT[i][64:70, :], 1.0), writes=[r_QT[i]])

                def load_head(h):
                    b = h % 2
                    kd = kv_dst[li][h]
                    nk = dk
                    P.dma("sp", KT[b][0:nk, :].rearrange("p (r t) -> p r t", r=4),
                          dap(kd, 0, [[T, nk], [rows * T, 4], [1, T]]), f"ldk{b}",
                          reads=[r_kvd[h]], writes=[r_KT[b]])
                    P.dma("sp", VV[b][:].rearrange("p (r l) c -> p r (l c)", r=4),
                          dap(kd, (rows - 65) * T, [[NBLK * 65, 128], [rows * T, 4], [1, NBLK * 65]]), f"ldv{b}",
                          reads=[r_kvd[h]], writes=[r_VV[b]])
                    P.dma("sp", QT[b][0:dk, :], qT_d[li][h, 0:dk, :], f"ldq{b}", reads=[r_qTd], writes=[r_QT[b]])
                    if li == 0:
                        P.dma("sp", KT[b][64:67, :], augk_d[h], f"ldk{b}", reads=[r_augk], writes=[r_KT[b]], merge=True)
                        P.dma("sp", QT[b][67:70, :], augq_d[h], f"ldq{b}", reads=[r_augq], writes=[r_QT[b]], merge=True)

                items = []
                for h in range(H):
                    for m in range(NM):
                        units = [(mp, rp, tp, 0) for mp in range(m) for rp in range(4) for tp in range(2)]
                        units += [(m, rp, 0, 0) for rp in range(4)]
                        units += [(m, rp, 1, 128) for rp in range(4)]
                        prs, cur, off = [], [], 0
                        for (mp, rp, tp, q0) in units:
                            w_ = 256 - q0
                            if off + w_ > 1024:
                                prs.append(cur)
                                cur, off = [], 0
                            cur.append((mp, rp, tp, q0, off))
                            off += w_
                        prs.append(cur)
                        for pi, pr in enumerate(prs):
                            items.append((h, m, pi, pr, pi == 0, pi == len(prs) - 1))
                state = {"n": 0}

                def emit_exp_pv(idx):
                    h, m, pi, pr, first, last = items[idx]
                    b = h % 2
                    sti = idx % 3
                    pt = idx % 3
                    ob = OB[(h * NM + m) % 2]
                    n = sum(256 - q0 for (_, _, _, q0, _) in pr)
                    P.op("act", lambda e, sti=sti, pt=pt, n=n: e.activation(
                        out=PT[pt][:, 0:n], in_=SBIG[sti][:, 0:n], func=ACT.Exp, scale=scale),
                        reads=r_S[sti], writes=[r_PT[pt]])
                    nu = len(pr)
                    for u, (mp, rp, tp, q0, off) in enumerate(pr):
                        kb = rp * NBLK + 2 * mp + tp
                        st_ = first and u == 0
                        fin = last and u == nu - 1
                        assert not (st_ and q0)
                        P.op("pe", lambda e, q0=q0, off=off, kb=kb, ob=ob, pt=pt, b=b, st_=st_, fin=fin: e.matmul(
                            psum[ob][0:65, q0:256], lhsT=VV[b][:, kb, :],
                            rhs=PT[pt][:, off:off + 256 - q0], start=st_, stop=fin),
                            reads=[r_PT[pt], r_VV[b]], writes=[ps_r[ob]], signal=(u == nu - 1))
                    if pending:
                        finalize(*pending.pop())
                    if last:
                        ou = fin_ctr["i"] % 2
                        fin_ctr["i"] += 1
                        P.op("dve", lambda e, ob=ob, ou=ou: e.tensor_copy(out=osb[ou][:], in_=psum[ob][0:65, 0:256]),
                             reads=[ps_r[ob]], writes=[r_osb[ou]])
                        pending.append((h, m, ou))

                def finalize(h, m, ou):
                    tb = TB[ou]
                    for tq in range(2):
                        P.op("pe", lambda e, tq=tq, ou=ou, tb=tb: e.transpose(
                            out=psum[tb][:, tq * 65:(tq + 1) * 65], in_=osb[ou][:, tq * 128:(tq + 1) * 128],
                            identity=identf[0:65, 0:65]),
                            reads=[r_osb[ou], r_const], writes=[ps_r[tb]], signal=(tq == 1))
                    P.op("dve", lambda e, tb=tb: e.reciprocal(
                        out=rec[:], in_=psum[tb][:, 0:130].rearrange("p (t c) -> p t c", t=2)[:, :, 64]),
                        reads=[ps_r[tb]], writes=[r_rec])
                    for tq in range(2):
                        l = 2 * m + tq
                        P.op("dve", lambda e, tq=tq, l=l, tb=tb, h=h: e.tensor_scalar(
                            out=o_all[:, l, h * 64:(h + 1) * 64], in0=psum[tb][:, tq * 65:tq * 65 + 64],
                            scalar1=rec[:, tq:tq + 1], scalar2=None, op0=ALU.mult),
                            reads=[ps_r[tb], r_rec], writes=[r_o])

                def emit_qk_sig(idx):
                    h, m, pi, pr, first, last = items[idx]
                    b = h % 2
                    sti = idx % 3
                    ninst = sum(2 if mp == m else 1 for (mp, rp, tp, q0, off) in pr)
                    k = 0
                    for u, (mp, rp, tp, q0, off) in enumerate(pr):
                        kcol = rp * T + (2 * mp + tp) * 128
                        diag = (mp == m)
                        w_ = 256 - q0
                        k += 1
                        P.op("pe", lambda e, off=off, w_=w_, q0=q0, kcol=kcol, diag=diag, sti=sti, b=b, m=m: e.matmul(
                            SBIG[sti][:, off:off + w_], lhsT=KT[b][:, kcol:kcol + 128],
                            rhs=QT[b][:, m * 256 + q0:(m + 1) * 256], start=True, stop=(not diag)),
                            reads=[r_KT[b], r_QT[b]], writes=r_S[sti], signal=(k == ninst))
                        if diag:
                            j = rp * 2 + tp
                            k += 1
                            P.op("pe", lambda e, off=off, w_=w_, q0=q0, j=j, sti=sti: e.matmul(
                                SBIG[sti][:, off:off + w_], lhsT=identb[:], rhs=maskb[:, j, q0:256],
                                start=False, stop=True),
                                reads=[r_const], writes=r_S[sti], signal=(k == ninst))

                loaded = set()

                def ensure_loaded(h):
                    if h < H and h not in loaded:
                        loaded.add(h)
                        load_head(h)

                ensure_loaded(0)
                ensure_loaded(1)
                LOOK = 2
                nit = len(items)
                for idx in range(min(LOOK, nit)):
                    emit_qk_sig(idx)
                for idx in range(nit):
                    emit_exp_pv(idx)
                    if idx + LOOK < nit:
                        hn = items[idx + LOOK][0]
                        emit_qk_sig(idx + LOOK)
                    h, m, pi, pr, first, last = items[idx]
                    if last and m == NM - 1:
                        ensure_loaded(h + 2)
                while pending:
                    finalize(*pending.pop())
                P.barrier()

        def out_proj(li, wout_d, o_all, r_o):
            with contextlib.ExitStack() as so:
                wo = sb("wo", [128, 8, D], BF16, stack=so)
                oT = [sb(f"oT{i}", [128, 8, 128], BF16, stack=so) for i in range(2)]
                r_wo = Res("wo")
                r_oT = [Res(f"oT{i}") for i in range(2)]
                P.dma("pool", wo[:], wout_d.ap().rearrange("(kc p) n -> p kc n", p=128), "ldwo", writes=[r_wo])
                for kc in range(8):
                    P.op("dve", lambda e, kc=kc: e.tensor_tensor(out=wo[:, kc, :], in0=wo[:, kc, :],
                                                                in1=modbc[:, 2 * D:3 * D], op=ALU.mult),
                         reads=[r_wo, r_mod], writes=[r_wo])
                def emit_T(l):
                    b = l % 2
                    tb = 5 + b
                    pT = psum[tb][:].bitcast(BF16)
                    for kc in range(8):
                        P.op("pe", lambda e, kc=kc, l=l, pT=pT: e.transpose(
                            out=pT[:, kc * 128:(kc + 1) * 128], in_=o_all[:, l, kc * 128:(kc + 1) * 128],
                            identity=identb[:]),
                            reads=[r_o, r_const], writes=[ps_r[tb]], signal=(kc == 7))
                    P.op("act", lambda e, b=b, pT=pT: e.activation(
                        out=oT[b][:], in_=pT[:, 0:1024].rearrange("p (k t) -> p k t", k=8), func=ACT.Copy),
                        reads=[ps_r[tb]], writes=[r_oT[b]])

                def emit_MM(l):
                    b = l % 2
                    yb = 0 if b == 0 else 2
                    for half in range(2):
                        for kc in range(8):
                            P.op("pe", lambda e, kc=kc, half=half, b=b, yb=yb: e.matmul(
                                psum[yb + half][:, :], lhsT=oT[b][:, kc, :], rhs=wo[:, kc, half * 512:(half + 1) * 512],
                                start=(kc == 0), stop=(kc == 7)),
                                reads=[r_oT[b], r_wo], writes=[ps_r[yb + half]], signal=(kc == 7))
                        P.op("dve", lambda e, half=half, l=l, yb=yb: e.tensor_tensor(
                            out=x_sb[:, l, half * 512:(half + 1) * 512], in0=x_sb[:, l, half * 512:(half + 1) * 512],
                            in1=psum[yb + half][:, :], op=ALU.add),
                            reads=[ps_r[yb + half], x_r[l]], writes=[x_r[l]])

                emit_T(0)
                for l in range(NBLK):
                    if l + 1 < NBLK:
                        emit_T(l + 1)
                    emit_MM(l)
                P.barrier()

        def moe(li):
            with contextlib.ExitStack() as sm:
                hT2 = sb("hT2", [128, NBLK, 8, 128], BF16, stack=sm)
                r_hT2 = [Res(f"hT2_{l}") for l in range(NBLK)]
                comb = sb("comb", [128, NBLK, E], stack=sm)
                r_comb = Res("comb")
                ss = sb("ss2", [128, NBLK], stack=sm)
                rstd = sb("rstd2", [128, NBLK], stack=sm)
                r_ss = Res("ss2")
                wgu = [sb("wgu0", [128, 8, 2, 2 * F], BF16, stack=sm), None]
                wdn = [sb("wdn0", [128, 2, 2, D], BF16, stack=sm), None]
                r_wgu = [Res(f"wgu{i}") for i in range(2)]
                r_wdn = [Res(f"wdn{i}") for i in range(2)]

                def load_pair_dma(ep):
                    b = ep % 2
                    for e2 in range(2):
                        ee = li * E + ep * 2 + e2
                        P.dma("pool", wgu[b][:, :, e2, 0:F], wg_d[ee].rearrange("(kc p) f -> p kc f", p=128),
                              writes=[r_wgu[b]], merge=(e2 > 0))
                        P.dma("pool", wgu[b][:, :, e2, F:2 * F], wu_d[ee].rearrange("(kc p) f -> p kc f", p=128),
                              writes=[r_wgu[b]], merge=True)
                        P.dma("pool", wdn[b][:, e2, :, :], wd_d[ee].rearrange("(fc p) d -> p fc d", p=128),
                              writes=[r_wdn[b]], merge=(e2 > 0))

                def load_pair_scale(ep):
                    b = ep % 2
                    for e2 in range(2):
                        for fc in range(2):
                            P.op("pool", lambda e, e2=e2, fc=fc, b=b: e.tensor_tensor(
                                out=wdn[b][:, e2, fc, :], in0=wdn[b][:, e2, fc, :], in1=modbc[:, 5 * D:6 * D],
                                op=ALU.mult),
                                reads=[r_wdn[b], r_mod], writes=[r_wdn[b]])

                def load_pair(ep):
                    load_pair_dma(ep)
                    load_pair_scale(ep)

                load_pair_dma(0)
                with contextlib.ExitStack() as s1:
                    junk = sb("junk2", [128, D], stack=s1)
                    r_junk = Res("junk2")
                    tmpn = sb("tmpn2", [128, D], stack=s1)
                    r_tmpn = Res("tmpn2")
                    h32 = [sb(f"h32b_{i}", [128, D], stack=s1) for i in range(2)]
                    r_h32 = [Res(f"h32b_{i}") for i in range(2)]
                    hT32 = [sb(f"hT32_{i}", [128, 8, 128], stack=s1) for i in range(2)]
                    r_hT32 = [Res(f"hT32_{i}") for i in range(2)]
                    wr = sb("wr", [128, 8, NRP], stack=s1)
                    brb = sb("brb", [128, NRP], stack=s1)
                    r_wr = Res("wr")
                    LG = sb("LG", [128, NBLK, NRP], stack=s1)
                    r_LG = Res("LG")
                    if "wr" not in os.environ.get("MK_SKIP", ""):
                        P.dma("sp", wr[:], wrt_d[li].rearrange("(kc p) n -> p kc n", p=128), "ldwr", writes=[r_wr])
                    if "br" not in os.environ.get("MK_SKIP", ""):
                        P.dma("sp", brb[:], brt_d[li:li + 1, :].partition_broadcast(128), "ldwr", writes=[r_wr])
                    norm_mod(li, 2, ffnn_d, None, tmpn, r_tmpn)
                    sumsq_blocks(ss, r_ss, junk, r_junk)
                    rstd_all(ss[:], rstd[:], r_ss, D)
                    def pre_blk(l):
                        b = l % 2
                        yield from make_hT(l, 3, 4, rstd, r_ss, h32[b], r_h32[b], hT2[:, l], r_hT2[l],
                                           (0 + 2 * b, 1 + 2 * b), hT32=hT32[b], r_hT32=r_hT32[b])
                        lb = 4 + b
                        for kc in range(8):
                            P.op("pe", lambda e, kc=kc, b=b, lb=lb: e.matmul(
                                psum[lb][:, 0:NRP], lhsT=hT32[b][:, kc, :], rhs=wr[:, kc, :],
                                start=(kc == 0), stop=(kc == 7)),
                                reads=[r_hT32[b], r_wr], writes=[ps_r[lb]], signal=(kc == 7))
                        P.op("dve", lambda e, l=l, lb=lb: e.tensor_tensor(
                            out=LG[:, l, :], in0=psum[lb][:, 0:NRP], in1=brb[:], op=ALU.add),
                            reads=[ps_r[lb], r_wr], writes=[r_LG], extra=())
                        yield

                    for l in range(0, NBLK, 2):
                        run_interleaved([pre_blk(l), pre_blk(l + 1)])
                    if stop_point("logits", P):
                        return
                    gl = LG[:, :, 0:G]
                    el = LG[:, :, G:NR].rearrange("p l (g e) -> p l g e", g=G)
                    gmax = sb("gmax", [128, NBLK], stack=s1)
                    gexp = sb("gexp", [128, NBLK, G], stack=s1)
                    gsum = sb("gsum", [128, NBLK], stack=s1)
                    gm = sb("gm", [128, NBLK, G], stack=s1)
                    esel = sb("esel", [128, NBLK, G, EPG], stack=s1)
                    ing = sb("ing", [128, NBLK, EPG], stack=s1)
                    m1 = sb("m1", [128, NBLK], stack=s1)
                    k1 = sb("k1", [128, NBLK, EPG], stack=s1)
                    ing2 = sb("ing2", [128, NBLK, EPG], stack=s1)
                    m2 = sb("m2", [128, NBLK], stack=s1)
                    k2 = sb("k2", [128, NBLK, EPG], stack=s1)
                    w1 = sb("w1", [128, NBLK], stack=s1)
                    w2 = sb("w2", [128, NBLK], stack=s1)
                    cig = sb("cig", [128, NBLK, EPG], stack=s1)
                    r_rt = Res("rt")

                    def dv(fn, reads=(), writes=()):
                        P.op("dve", fn, reads=[r_LG, r_rt] + list(reads), writes=[r_rt] + list(writes))

                    def bcl(ap2, n):
                        return ap2.unsqueeze(2).to_broadcast([128, NBLK, n])

                    dv(lambda e: e.tensor_reduce(out=gmax[:], in_=gl, axis=AX.X, op=ALU.max))
                    dv(lambda e: e.tensor_tensor(out=gexp[:], in0=gl, in1=bcl(gmax[:], G), op=ALU.subtract))
                    dv(lambda e: e.tensor_tensor(out=gm[:], in0=gl, in1=bcl(gmax[:], G), op=ALU.is_ge))
                    P.op("act", lambda e: e.activation(out=gexp[:], in_=gexp[:], func=ACT.Exp),
                         reads=[r_rt], writes=[r_rt])
                    dv(lambda e: e.tensor_reduce(out=gsum[:], in_=gexp[:], axis=AX.X, op=ALU.add))
                    dv(lambda e: e.reciprocal(out=gsum[:], in_=gsum[:]))
                    dv(lambda e: e.tensor_tensor(out=esel[:], in0=el,
                                                 in1=gm[:].unsqueeze(3).to_broadcast([128, NBLK, G, EPG]),
                                                 op=ALU.mult))
                    dv(lambda e: e.tensor_reduce(out=ing[:], in_=esel[:].rearrange("p l g e -> p l e g"),
                                                 axis=AX.X, op=ALU.add))
                    dv(lambda e: e.tensor_reduce(out=m1[:], in_=ing[:], axis=AX.X, op=ALU.max))
                    dv(lambda e: e.tensor_tensor(out=k1[:], in0=ing[:], in1=bcl(m1[:], EPG), op=ALU.is_ge))
                    dv(lambda e: e.scalar_tensor_tensor(out=ing2[:], in0=k1[:], scalar=-1e30, in1=ing[:],
                                                        op0=ALU.mult, op1=ALU.add))
                    dv(lambda e: e.tensor_reduce(out=m2[:], in_=ing2[:], axis=AX.X, op=ALU.max))
                    dv(lambda e: e.tensor_tensor(out=k2[:], in0=ing2[:], in1=bcl(m2[:], EPG), op=ALU.is_ge))
                    dv(lambda e: e.tensor_tensor(out=w1[:], in0=m2[:], in1=m1[:], op=ALU.subtract))
                    P.op("act", lambda e: e.activation(out=w1[:], in_=w1[:], func=ACT.Exp), reads=[r_rt], writes=[r_rt])
                    dv(lambda e: e.tensor_scalar(out=w1[:], in0=w1[:], scalar1=1.0, scalar2=None, op0=ALU.add))
                    dv(lambda e: e.reciprocal(out=w1[:], in_=w1[:]))
                    dv(lambda e: e.tensor_scalar(out=w2[:], in0=w1[:], scalar1=-1.0, scalar2=1.0, op0=ALU.mult,
                                                 op1=ALU.add))
                    dv(lambda e: e.tensor_tensor(out=w1[:], in0=w1[:], in1=gsum[:], op=ALU.mult))
                    dv(lambda e: e.tensor_tensor(out=w2[:], in0=w2[:], in1=gsum[:], op=ALU.mult))
                    dv(lambda e: e.tensor_tensor(out=k1[:], in0=k1[:], in1=bcl(w1[:], EPG), op=ALU.mult))
                    dv(lambda e: e.tensor_tensor(out=k2[:], in0=k2[:], in1=bcl(w2[:], EPG), op=ALU.mult))
                    dv(lambda e: e.tensor_tensor(out=cig[:], in0=k1[:], in1=k2[:], op=ALU.add))
                    dv(lambda e: e.tensor_tensor(out=comb[:].rearrange("p l (g e) -> p l g e", g=G),
                                                 in0=gm[:].unsqueeze(3).to_broadcast([128, NBLK, G, EPG]),
                                                 in1=cig[:].unsqueeze(2).to_broadcast([128, NBLK, G, EPG]),
                                                 op=ALU.mult), writes=[r_comb])
                    P.barrier()
                if stop_point("route", P):
                    return
                with contextlib.ExitStack() as s2:
                    wgu[1] = sb("wgu1", [128, 8, 2, 2 * F], BF16, stack=s2)
                    wdn[1] = sb("wdn1", [128, 2, 2, D], BF16, stack=s2)
                    sa_t = [sb(f"sa{i}", [128, F], stack=s2) for i in range(2)]
                    hid = [sb(f"hid{i}", [128, F], BF16, stack=s2) for i in range(2)]
                    hidT = [sb(f"hidT{i}", [128, 2, 128], BF16, stack=s2) for i in range(2)]
                    r_sa = [Res(f"sa{i}") for i in range(2)]
                    r_hid = [Res(f"hid{i}") for i in range(2)]
                    r_hidT = [Res(f"hidT{i}") for i in range(2)]
                    NP = E // 2
                    load_pair_scale(0)
                    if stop_point("wload", P):
                        return
                    units = [(ep, l, e2) for ep in range(NP) for l in range(NBLK) for e2 in range(2)]
                    NU = len(units)
                    GUB = [0, 1, 2]
                    TTB = 3

                    def emit_gu(i):
                        ep, l, e2 = units[i]
                        b = ep % 2
                        gub = GUB[i % 3]
                        for kc in range(8):
                            P.op("pe", lambda e, kc=kc, l=l, e2=e2, b=b, gub=gub: e.matmul(
                                psum[gub][:, :], lhsT=hT2[:, l, kc, :], rhs=wgu[b][:, kc, e2, :],
                                start=(kc == 0), stop=(kc == 7)),
                                reads=[r_hT2[l], r_wgu[b]], writes=[ps_r[gub]], signal=(kc == 7))

                    def emit_rest(i):
                        ep, l, e2 = units[i]
                        b = ep % 2
                        ee = ep * 2 + e2
                        g = i % 2
                        gub = GUB[i % 3]
                        yb = 4 + 2 * (l % 2)
                        P.op("act", lambda e, g=g, gub=gub: e.activation(
                            out=sa_t[g][:], in_=psum[gub][:, 0:F], func=ACT.Silu),
                            reads=[ps_r[gub]], writes=[r_sa[g]])
                        P.op("dve", lambda e, g=g, gub=gub, l=l, ee=ee: e.scalar_tensor_tensor(
                            out=hid[g][:], in0=psum[gub][:, F:2 * F], scalar=comb[:, l, ee:ee + 1],
                            in1=sa_t[g][:], op0=ALU.mult, op1=ALU.mult),
                            reads=[ps_r[gub], r_comb, r_sa[g]], writes=[r_hid[g]])
                        pT = psum[TTB][:].bitcast(BF16)
                        for fc in range(2):
                            P.op("pe", lambda e, fc=fc, g=g, pT=pT: e.transpose(
                                out=pT[:, fc * 128:(fc + 1) * 128], in_=hid[g][:, fc * 128:(fc + 1) * 128],
                                identity=identb[:]),
                                reads=[r_hid[g], r_const], writes=[ps_r[TTB]], signal=(fc == 1))
                        if i + 2 < NU:
                            emit_gu(i + 2)
                        P.op("act", lambda e, g=g, pT=pT: e.activation(
                            out=hidT[g][:], in_=pT[:, 0:256].rearrange("p (f t) -> p f t", f=2),
                            func=ACT.Copy),
                            reads=[ps_r[TTB]], writes=[r_hidT[g]])
                        for fc in range(2):
                            for half in range(2):
                                first = (e2 == 0 and fc == 0)
                                lastm = (e2 == 1 and fc == 1)
                                P.op("pe", lambda e, fc=fc, half=half, g=g, b=b, e2=e2, yb=yb, first=first,
                                     lastm=lastm: e.matmul(
                                    psum[yb + half][:, :], lhsT=hidT[g][:, fc, :],
                                    rhs=wdn[b][:, e2, fc, half * 512:(half + 1) * 512],
                                    start=first, stop=lastm),
                                    reads=[r_hidT[g], r_wdn[b]], writes=[ps_r[yb + half]],
                                    signal=(fc == 1 and half == 1))
                        if e2 == 1:
                            for half in range(2):
                                P.op("dve", lambda e, half=half, l=l, yb=yb: e.tensor_tensor(
                                    out=x_sb[:, l, half * 512:(half + 1) * 512],
                                    in0=x_sb[:, l, half * 512:(half + 1) * 512], in1=psum[yb + half][:, :],
                                    op=ALU.add),
                                    reads=[ps_r[yb + half], x_r[l]], writes=[x_r[l]])

                    emit_gu(0)
                    if NU > 1:
                        emit_gu(1)
                    for i in range(NU):
                        ep, l, e2 = units[i]
                        if l == 0 and e2 == 0 and ep + 1 < NP:
                            load_pair(ep + 1)
                        emit_rest(i)
                    P.barrier()

        r_qTd = Res("qTd")
        r_kvs = [Res(f"kvs{h}") for h in range(H)]
        r_kvd = [Res(f"kvd{h}") for h in range(H)]
        r_augk = Res("augk")
        r_augq = Res("augq")

        def fox_layer(li):
            load_mod(li)
            with contextlib.ExitStack() as sl:
                lf = sb("lf", [128, NBLK, H], stack=sl)
                r_lf = Res("lf")
                with contextlib.ExitStack() as s1:
                    win = sb("win", [128, 8, cfg.FOX_IN], BF16, stack=s1)
                    r_win = Res("win")
                    ss = sb("ss", [128, NBLK], stack=s1)
                    rstd = sb("rstd", [128, NBLK], stack=s1)
                    r_ss = Res("ss")
                    h32 = [sb(f"h32_{i}", [128, D], stack=s1) for i in range(2)]
                    r_h32 = [Res(f"h32_{i}") for i in range(2)]
                    hT = [sb(f"hT_{i}", [128, 8, 128], BF16, stack=s1) for i in range(2)]
                    r_hT = [Res(f"hT_{i}") for i in range(2)]
                    qk32s = [[sb(f"qk32_{j}_{i}", [128, D], stack=s1) for i in range(2)] for j in range(2)]
                    r_qk32s = [[Res(f"qk32_{j}_{i}") for i in range(2)] for j in range(2)]
                    qk32, r_qk32 = qk32s[0], r_qk32s[0]
                    sq = sb("sq", [128, D], stack=s1)
                    r_sq = Res("sq")
                    ssqs = [sb(f"ssq{i}", [128, 2, H], stack=s1) for i in range(2)]
                    r_ssqs = [Res(f"ssq{i}") for i in range(2)]
                    rsq = sb("rsq", [128, 2, H], stack=s1)
                    r_rsq = Res("rsq")
                    gq = sb("gq", [128, 2, 64], stack=s1)
                    r_gq = Res("gq")
                    qkn = [sb(f"qkn_{i}", [128, D], BF16, stack=s1) for i in range(2)]
                    r_qkn = [Res(f"qkn_{i}") for i in range(2)]
                    qkT = [sb(f"qkT_{i}", [64, H, 128], BF16, stack=s1) for i in range(2)]
                    r_qkT = [Res(f"qkT_{i}") for i in range(2)]
                    vsts = [sb(f"vst{i}", [128, H, 65], BF16, stack=s1) for i in range(2)]
                    r_vsts = [Res(f"vst{i}") for i in range(2)]
                    bfb = sb("bfb", [128, H], stack=s1)
                    zfs = [sb(f"zf{i}", [128, H], stack=s1) for i in range(2)]
                    r_zfs = [Res(f"zf{i}") for i in range(2)]
                    tmpn, r_tmpn = sq, r_sq
                    junk, r_junk = qk32[0], r_qk32[0]

                    P.dma("pool", win[:], fox_win_d.ap().rearrange("(kc p) n -> p kc n", p=128), "ldwin",
                          writes=[r_win])
                    P.dma("sp", gq[:, 0, :], fox_qn_d[0:1, :].partition_broadcast(128), "ldn", writes=[r_gq])
                    P.dma("sp", gq[:, 1, :], fox_kn_d[0:1, :].partition_broadcast(128), "ldn", writes=[r_gq])
                    P.dma("sp", bfb[:], fox_bf_d[0:1, :].partition_broadcast(128), "ldn", writes=[r_gq])
                    for i_ in range(2):
                        P.op("pool", lambda e, i_=i_: e.memset(vsts[i_][:, :, 64:65], 1.0), writes=[r_vsts[i_]])
                    norm_mod(li, 1, mixn_d, None, tmpn, r_tmpn)
                    sumsq_blocks(ss, r_ss, junk, r_junk)
                    rstd_all(ss[:], rstd[:], r_ss, D)
                    def blockA(l):
                        b = l % 2
                        qk32, r_qk32, ssq, r_ssq = qk32s[b], r_qk32s[b], ssqs[b], r_ssqs[b]
                        vst, r_vst, zf, r_zf = vsts[b], r_vsts[b], zfs[b], r_zfs[b]
                        yield from make_hT(l, 0, 1, rstd, r_ss, h32[b], r_h32[b], hT[b], r_hT[b], (0, 1))
                        for ci in range(7):
                            c0 = ci * 512
                            c1 = min(cfg.FOX_IN, c0 + 512)
                            bank = 2 + (ci % 4)
                            for kc in range(8):
                                P.op("pe", lambda e, kc=kc, c0=c0, c1=c1, bank=bank, b=b: e.matmul(
                                    psum[bank][:, 0:c1 - c0], lhsT=hT[b][:, kc, :], rhs=win[:, kc, c0:c1],
                                    start=(kc == 0), stop=(kc == 7)),
                                    reads=[r_hT[b], r_win], writes=[ps_r[bank]], signal=(kc == 7))
                            if ci < 4:
                                w = ci // 2
                                hf = ci % 2
                                P.op("act", lambda e, w=w, hf=hf, bank=bank: e.activation(
                                    out=qk32[w][:, hf * 512:(hf + 1) * 512], in_=psum[bank][:, :], func=ACT.Copy),
                                    reads=[ps_r[bank]], writes=[r_qk32[w]])
                                P.op("act", lambda e, w=w, hf=hf, bank=bank: e.activation(
                                    out=sq[:, hf * 512:(hf + 1) * 512], in_=psum[bank][:, :], func=ACT.Square),
                                    reads=[ps_r[bank]], writes=[r_sq])
                                P.op("dve", lambda e, w=w, hf=hf: e.tensor_reduce(
                                    out=ssq[:, w, hf * 8:(hf + 1) * 8],
                                    in_=sq[:, hf * 512:(hf + 1) * 512].rearrange("p (h d) -> p h d", d=64),
                                    axis=AX.X, op=ALU.add),
                                    reads=[r_sq], writes=[r_ssq])
                            elif ci < 6:
                                hf = ci - 4
                                P.op("act", lambda e, hf=hf, bank=bank: e.activation(
                                    out=vst[:, hf * 8:(hf + 1) * 8, 0:64],
                                    in_=psum[bank][:, :].rearrange("p (h d) -> p h d", d=64), func=ACT.Copy),
                                    reads=[ps_r[bank]], writes=[r_vst])
                            else:
                                P.op("dve", lambda e, bank=bank: e.tensor_tensor(
                                    out=zf[:], in0=psum[bank][:, 0:H], in1=bfb[:], op=ALU.add),
                                    reads=[ps_r[bank], r_gq], writes=[r_zf])
                            yield

                    def blockB(l):
                        b = l % 2
                        qk32, r_qk32, ssq, r_ssq = qk32s[b], r_qk32s[b], ssqs[b], r_ssqs[b]
                        vst, r_vst, zf, r_zf = vsts[b], r_vsts[b], zfs[b], r_zfs[b]
                        P.op("act", lambda e: e.activation(out=zf[:], in_=zf[:], func=ACT.Exp, scale=-1.0),
                             reads=[r_zf], writes=[r_zf])
                        P.op("act", lambda e: e.activation(out=zf[:], in_=zf[:], func=ACT.Ln, bias=1.0),
                             reads=[r_zf], writes=[r_zf])
                        P.op("dve", lambda e, l=l: e.tensor_scalar(out=lf[:, l, :], in0=zf[:], scalar1=-1.0,
                                                                  scalar2=None, op0=ALU.mult),
                             reads=[r_zf], writes=[r_lf])
                        yield
                        P.op("act", lambda e: e.activation(out=rsq[:], in_=ssq[:], func=ACT.Sqrt, scale=1.0 / 64,
                                                           bias=EPS), reads=[r_ssq], writes=[r_rsq])
                        P.op("dve", lambda e: e.reciprocal(out=rsq[:], in_=rsq[:]), reads=[r_rsq], writes=[r_rsq])
                        yield
                        for w in range(2):
                            P.op("dve", lambda e, w=w: e.tensor_tensor(
                                out=qk32[w][:].rearrange("p (h d) -> p h d", d=64),
                                in0=qk32[w][:].rearrange("p (h d) -> p h d", d=64),
                                in1=rsq[:, w, :].unsqueeze(2).to_broadcast([128, H, 64]), op=ALU.mult),
                                reads=[r_qk32[w], r_rsq], writes=[r_qk32[w]])
                            yield
                            P.op("pool", lambda e, w=w: e.tensor_tensor(
                                out=qkn[w][:].rearrange("p (h d) -> p h d", d=64),
                                in0=qk32[w][:].rearrange("p (h d) -> p h d", d=64),
                                in1=gq[:, w, :].unsqueeze(1).to_broadcast([128, H, 64]), op=ALU.mult),
                                reads=[r_qk32[w], r_gq], writes=[r_qkn[w]])
                            yield
                            for hb in range(2):
                                tbk = 6 + hb
                                pT = psum[tbk][:].bitcast(BF16)
                                for h8 in range(8):
                                    hh = hb * 8 + h8
                                    P.op("pe", lambda e, w=w, hh=hh, h8=h8, pT=pT: e.transpose(
                                        out=pT[0:64, h8 * 128:(h8 + 1) * 128], in_=qkn[w][:, hh * 64:(hh + 1) * 64],
                                        identity=identb[:]),
                                        reads=[r_qkn[w], r_const], writes=[ps_r[tbk]], signal=(h8 == 7))
                                P.op("act", lambda e, w=w, hb=hb, pT=pT: e.activation(
                                    out=qkT[w][:, hb * 8:(hb + 1) * 8, :],
                                    in_=pT[0:64, 0:1024].rearrange("p (h t) -> p h t", h=8), func=ACT.Copy),
                                    reads=[ps_r[tbk]], writes=[r_qkT[w]])
                                yield
                        P.dma("sp", dap(qT_d[li], l * 128, [[T, 64], [DKMAX * T, H], [1, 128]]), qkT[0][:],
                              "stq", reads=[r_qkT[0]], writes=[r_qTd], merge=True)
                        for h in range(H):
                            P.dma("sp", kv_src[li][h][0:64, l * 128:(l + 1) * 128], qkT[1][:, h, :], "stk",
                                  reads=[r_qkT[1]], writes=[r_kvs[h]], merge=True)
                            P.dma("sp", dap(kv_src[li][h], 64 * T + l * 65, [[NBLK * 65, 128], [1, 65]]),
                                  vst[:, h, :], "stv", reads=[r_vst], writes=[r_kvs[h]], merge=True)
                        yield

                    def run_interleaved(gens):
                        gens = list(gens)
                        while gens:
                            for g_ in list(gens):
                                try:
                                    next(g_)
                                except StopIteration:
                                    gens.remove(g_)

                    run_interleaved([blockA(0)])
                    for l in range(NBLK):
                        gl_ = [blockA(l + 1)] if l + 1 < NBLK else []
                        run_interleaved(gl_ + [blockB(l)])
                    P.barrier()
                    if stop_point("proj", P):
                        return
                with contextlib.ExitStack() as s1:
                    r_lfs = Res("lf_src")
                    r_lfd = Res("lf_dst")
                    P.dma("sp", lf_src.ap(), lf[:].rearrange("p l h -> p (l h)"), "stlf", reads=[r_lf], writes=[r_lfs])
                    P.collective(cfg.groups, lf_src.ap().opt(), lf_dst.ap().opt(), "cc_lf", reads=[r_lfs],
                                 writes=[r_lfd])
                    for h in range(H):
                        P.collective(cfg.groups, kv_src[li][h].ap().opt(), kv_dst[li][h].ap().opt(), f"cc_kv{h % 4}",
                                     reads=[r_kvs[h]], writes=[r_kvd[h]])
                    NK = 4 * NBLK
                    L = sb("Lall", [128, NK, H], stack=s1)
                    W = sb("Wall", [128, NK, H], stack=s1)
                    tot = sb("tot", [128, H, 8 * NM], stack=s1)
                    pre = sb("pre", [128, H, 8 * NM], stack=s1)
                    onesrow = sb("onesrow", [128, 8 * NM], stack=s1)
                    ownc = sb("ownc", [128, NBLK, H], stack=s1)
                    c3 = sb("c3", [128, NBLK, H, 3], BF16, stack=s1)
                    r1 = sb("r1", [128, NBLK, H], stack=s1)
                    hi = sb("hi", [128, NBLK, H], BF16, stack=s1)
                    augT = sb("augT", [48, NBLK, 128], BF16, stack=s1)
                    r_L = Res("Lall")
                    r_cs = Res("cs")
                    r_aug = Res("augT")
                    P.dma("sp", L[:].rearrange("p (r l) h -> p r (l h)", r=4),
                          dap(lf_dst, 0, [[NBLK * H, 128], [128 * NBLK * H, 4], [1, NBLK * H]]), "ldlf",
                          reads=[r_lfd], writes=[r_L])
                    P.op("pool", lambda e: e.memset(onesrow[:], 1.0), writes=[r_cs])
                    Lf = L[:].rearrange("p k h -> p (k h)")
                    nch = (NK * H + 511) // 512
                    for ch in range(nch):
                        c0, c1 = ch * 512, min(NK * H, (ch + 1) * 512)
                        P.op("pe", lambda e, c0=c0, c1=c1: e.matmul(psum[0][:, 0:c1 - c0], lhsT=trif[:], rhs=Lf[:, c0:c1],
                                                                    start=True, stop=True),
                             reads=[r_L, r_const], writes=[ps_r[0]])
                        P.op("pe", lambda e, c0=c0, c1=c1: e.matmul(psum[1][:, 0:c1 - c0], lhsT=onesf[:], rhs=Lf[:, c0:c1],
                                                                    start=True, stop=True),
                             reads=[r_L, r_const], writes=[ps_r[1]])
                        P.op("dve", lambda e, c0=c0, c1=c1: e.tensor_copy(
                            out=W[:].rearrange("p k h -> p (k h)")[:, c0:c1], in_=psum[0][:, 0:c1 - c0]),
                            reads=[ps_r[0]], writes=[r_cs])
                        nkb = (c1 - c0) // H
                        for kk in range(nkb):
                            kbi = c0 // H + kk
                            rp, lp = divmod(kbi, NBLK)
                            sbk = seq_block(rp, lp)
                            P.op("dve", lambda e, kk=kk, sbk=sbk: e.tensor_copy(
                                out=tot[:, :, sbk], in_=psum[1][:, kk * H:(kk + 1) * H]),
                                reads=[ps_r[1]], writes=[r_cs])
                    for h in range(H):
                        P.op("dve", lambda e, h=h: e.tensor_tensor_scan(
                            out=pre[:, h, :], data0=onesrow[:], data1=tot[:, h, :], initial=0.0,
                            op0=ALU.mult, op1=ALU.add), reads=[r_cs], writes=[r_cs])
                    P.op("dve", lambda e: e.tensor_tensor(out=pre[:], in0=pre[:], in1=tot[:], op=ALU.subtract),
                         reads=[r_cs], writes=[r_cs])
                    for kbi in range(NK):
                        rp, lp = divmod(kbi, NBLK)
                        sbk = seq_block(rp, lp)
                        P.op("dve", lambda e, kbi=kbi, sbk=sbk: e.tensor_tensor(
                            out=W[:, kbi, :], in0=W[:, kbi, :], in1=pre[:, :, sbk], op=ALU.add),
                            reads=[r_cs], writes=[r_cs])
                    Wr = W[:].rearrange("p (r l) h -> p r l h", r=4)
                    P.op("dve", lambda e: e.tensor_scalar(out=ownc[:], in0=Wr[:, 0], scalar1=sel_sb[:, 0:1],
                                                          scalar2=None, op0=ALU.mult),
                         reads=[r_cs, r_const], writes=[r_cs])
                    for rp in range(1, 4):
                        P.op("dve", lambda e, rp=rp: e.scalar_tensor_tensor(
                            out=ownc[:], in0=Wr[:, rp], scalar=sel_sb[:, rp:rp + 1], in1=ownc[:],
                            op0=ALU.mult, op1=ALU.add), reads=[r_cs, r_const], writes=[r_cs])
                    P.op("dve", lambda e: e.tensor_scalar(out=ownc[:], in0=ownc[:], scalar1=-8.0, scalar2=None,
                                                          op0=ALU.mult), reads=[r_cs], writes=[r_cs])
                    for j in range(3):
                        P.op("dve", lambda e, j=j: e.tensor_copy(out=c3[:, :, :, j], in_=ownc[:]),
                             reads=[r_cs], writes=[r_cs])
                        if j < 2:
                            P.op("dve", lambda e, j=j: e.tensor_tensor(out=ownc[:], in0=ownc[:], in1=c3[:, :, :, j],
                                                                      op=ALU.subtract), reads=[r_cs], writes=[r_cs])
                    for l in range(NBLK):
                        tbk = 2 + (l % 2)
                        pT = psum[tbk][:].bitcast(BF16)
                        P.op("pe", lambda e, l=l, pT=pT: e.transpose(
                            out=pT[0:48, 0:128], in_=c3[:, l].rearrange("p h j -> p (h j)"), identity=identb[:]),
                            reads=[r_cs, r_const], writes=[ps_r[tbk]])
                        P.op("act", lambda e, l=l, pT=pT: e.activation(out=augT[:, l, :], in_=pT[0:48, 0:128],
                                                                      func=ACT.Copy),
                             reads=[ps_r[tbk]], writes=[r_aug])
                    for h in range(H):
                        P.dma("sp", augq_d[h], augT[3 * h:3 * h + 3, :, :].rearrange("p l t -> p (l t)"),
                              "staug", reads=[r_aug], writes=[r_augq], merge=True)
                    c3a = sb("c3a", [128, NK, H, 3], BF16, stack=s1)
                    augTa = sb("augTa", [48, NK, 128], BF16, stack=s1)
                    r_auga = Res("augTa")
                    P.op("dve", lambda e: e.tensor_scalar(out=W[:], in0=W[:], scalar1=-8.0, scalar2=None,
                                                          op0=ALU.mult), reads=[r_cs], writes=[r_cs])
                    for j in range(3):
                        P.op("dve", lambda e, j=j: e.tensor_copy(out=c3a[:, :, :, j], in_=W[:]),
                             reads=[r_cs], writes=[r_cs])
                        if j < 2:
                            P.op("dve", lambda e, j=j: e.tensor_tensor(out=W[:], in0=W[:], in1=c3a[:, :, :, j],
                                                                      op=ALU.subtract), reads=[r_cs], writes=[r_cs])
                    for g8 in range(NK // 8):
                        tbk = 4 + (g8 % 2)
                        pT = psum[tbk][:].bitcast(BF16)
                        for k8 in range(8):
                            kb = g8 * 8 + k8
                            P.op("pe", lambda e, kb=kb, k8=k8, pT=pT: e.transpose(
                                out=pT[0:48, k8 * 128:(k8 + 1) * 128], in_=c3a[:, kb].rearrange("p h j -> p (h j)"),
                                identity=identb[:]),
                                reads=[r_cs, r_const], writes=[ps_r[tbk]], signal=(k8 == 7))
                        P.op("act", lambda e, g8=g8, pT=pT: e.activation(
                            out=augTa[:, g8 * 8:(g8 + 1) * 8, :],
                            in_=pT[0:48, 0:1024].rearrange("p (k t) -> p k t", k=8), func=ACT.Copy),
                            reads=[ps_r[tbk]], writes=[r_auga])
                    for h in range(H):
                        P.dma("sp", augk_d[h], augTa[3 * h:3 * h + 3, :, :].rearrange("p k t -> p (k t)"),
                              "staugk", reads=[r_auga], writes=[r_augk], merge=True)
                    if stop_point("cum", P):
                        return
                    P.barrier(skip_cc=True)
                if stop_point("coll", P):
                    return
                o_all = sb("o_all", [128, NBLK, D], BF16, stack=sl)
                r_o = Res("o_all")
                attention(li, 64, 70, 0.125, o_all, r_o)
                if stop_point("attn", P):
                    return
                out_proj(li, fox_wout_d, o_all, r_o)
            if stop_point("oproj", P):
                return
            moe(li)


        def mla_layer(li):
            DK = 96
            scale = 96.0 ** -0.5
            load_mod(li)
            with contextlib.ExitStack() as sl:
                with contextlib.ExitStack() as s1:
                    win = sb("mwin", [128, 8, 672], BF16, stack=s1)
                    wuq = sb("wuq", [128, 3, 1536], BF16, stack=s1)
                    wukv = sb("wukv", [128, 2, 2048], BF16, stack=s1)
                    r_w = Res("mla_w")
                    gl = sb("gl", [128, 640], stack=s1)
                    gqk = sb("gqk", [128, 2, 96], stack=s1)
                    invf = sb("invf_sb", [128, 16], stack=s1)
                    posi = sb("posi", [128, NBLK], I32, stack=s1)
                    posf = sb("posf", [128, NBLK], stack=s1)
                    ang = sb("ang", [128, 2, NBLK, 16], stack=s1)
                    kq = sb("kq", [128, 2, NBLK, 16], stack=s1)
                    kqi = sb("kqi", [128, 2, NBLK, 16], I32, stack=s1)
                    trig = sb("trig", [128, 2, NBLK, 16], stack=s1)
                    r_g = Res("mla_g")
                    r_trig = Res("trig")
                    ss = sb("ss1", [128, NBLK], stack=s1)
                    rstd = sb("rstd1", [128, NBLK], stack=s1)
                    r_ss = Res("ss1")
                    h32 = [sb(f"h32m_{i}", [128, D], stack=s1) for i in range(2)]
                    r_h32 = [Res(f"h32m_{i}") for i in range(2)]
                    hT = [sb(f"hTm_{i}", [128, 8, 128], BF16, stack=s1) for i in range(2)]
                    r_hT = [Res(f"hTm_{i}") for i in range(2)]
                    pj32 = sb("pj32", [128, 672], stack=s1)
                    r_pj = Res("pj32")
                    ssl = sb("ssl", [128, 4], stack=s1)
                    r_ssl = Res("ssl")
                    latn = sb("latn", [128, 640], BF16, stack=s1)
                    r_latn = Res("latn")
                    latT = sb("latT", [128, 5, 128], BF16, stack=s1)
                    r_latT = Res("latT")
                    QK32s = [sb(f"QK32_{i}", [128, 2, H, 96], stack=s1) for i in range(2)]
                    r_QKs = [Res(f"QK32_{i}") for i in range(2)]
                    SQ = sb("SQ", [128, 2, H, 96], stack=s1)
                    r_SQ = Res("SQ")
                    SQf = SQ[:].rearrange("p w h d -> p (w h d)")
                    tmpn, r_tmpn = SQf[:, 0:D], r_SQ
                    junk, r_junk = SQf[:, D:2 * D], r_SQ
                    junkA = sb("junkA", [128, 384], stack=s1)
                    r_junkA = Res("junkA")
                    rt = [SQf[:, i * 512:(i + 1) * 512].rearrange("p (a c) -> p a c", c=16) for i in range(4)]
                    ssqk = sb("ssqk", [128, 2, H], stack=s1)
                    rsqk = sb("rsqk", [128, 2, H], stack=s1)
                    r_ssqk = Res("ssqk")
                    r_rt = r_SQ
                    QKB = sb("QKB", [128, 2, H, 96], BF16, stack=s1)
                    r_QKB = Res("QKB")
                    stT0 = sb("stT0", [96, 2, H, 128], BF16, stack=s1)
                    stT = [stT0, stT0]
                    r_stT0 = Res("stT0")
                    r_stT = [r_stT0, r_stT0]
                    vsts = [sb(f"vst1_{i}", [128, H, 65], BF16, stack=s1) for i in range(2)]
                    r_vsts = [Res(f"vst1_{i}") for i in range(2)]

                    P.dma("pool", win[:], mla_win_d.ap().rearrange("(kc p) n -> p kc n", p=128), writes=[r_w])
                    P.dma("pool", wuq[:], mla_wuq_d.ap().rearrange("(kc p) n -> p kc n", p=128), writes=[r_w])
                    P.dma("pool", wukv[:], mla_wukv_d.ap().rearrange("(kc p) n -> p kc n", p=128), writes=[r_w])
                    P.dma("sp", gl[:, 0:384], mla_qln_d[0:1, :].partition_broadcast(128), writes=[r_g])
                    P.dma("sp", gl[:, 384:640], mla_kvln_d[0:1, :].partition_broadcast(128), writes=[r_g])
                    P.dma("sp", gqk[:, 0, :], mla_qn_d[0:1, :].partition_broadcast(128), writes=[r_g])
                    P.dma("sp", gqk[:, 1, :], mla_kn_d[0:1, :].partition_broadcast(128), writes=[r_g])
                    P.dma("sp", invf[:], invf_d[:, :], writes=[r_g])
                    P.dma("sp", posi[:], pos_d[:, :], writes=[r_g])
                    for i_ in range(2):
                        P.op("pool", lambda e, i_=i_: e.memset(vsts[i_][:, :, 64:65], 1.0), writes=[r_vsts[i_]])
                    TWO_PI = 6.283185307179586
                    C1 = 6.28125
                    C2 = TWO_PI - C1

                    def dt_(fn, reads=(), writes=()):
                        P.op("dve", fn, reads=[r_g, r_trig] + list(reads), writes=[r_trig] + list(writes))

                    dt_(lambda e: e.tensor_copy(out=posf[:], in_=posi[:]))
                    dt_(lambda e: e.tensor_tensor(out=ang[:, 0], in0=posf[:].unsqueeze(2).to_broadcast([128, NBLK, 16]),
                                                  in1=invf[:].unsqueeze(1).to_broadcast([128, NBLK, 16]), op=ALU.mult))
                    dt_(lambda e: e.tensor_scalar(out=ang[:, 1], in0=ang[:, 0], scalar1=TWO_PI / 4, scalar2=None,
                                                  op0=ALU.add))
                    dt_(lambda e: e.tensor_scalar(out=kq[:], in0=ang[:], scalar1=1.0 / TWO_PI, scalar2=None,
                                                  op0=ALU.mult))
                    dt_(lambda e: e.tensor_copy(out=kqi[:], in_=kq[:]))
                    dt_(lambda e: e.tensor_copy(out=kq[:], in_=kqi[:]))
                    dt_(lambda e: e.scalar_tensor_tensor(out=ang[:], in0=kq[:], scalar=-C1, in1=ang[:],
                                                         op0=ALU.mult, op1=ALU.add))
                    dt_(lambda e: e.scalar_tensor_tensor(out=ang[:], in0=kq[:], scalar=-C2, in1=ang[:],
                                                         op0=ALU.mult, op1=ALU.add))
                    dt_(lambda e: e.tensor_scalar(out=ang[:], in0=ang[:], scalar1=3.1415925, scalar2=-3.1415925,
                                                  op0=ALU.min, op1=ALU.max))
                    P.op("act", lambda e: e.activation(out=trig[:], in_=ang[:], func=ACT.Sin),
                         reads=[r_trig], writes=[r_trig])

                    norm_mod(li, 1, mixn_d, None, tmpn, r_tmpn)
                    sumsq_blocks(ss, r_ss, junk, r_junk)
                    rstd_all(ss[:], rstd[:], r_ss, D)
                    bank_ctr = {"i": 0}

                    def nb():
                        b_ = 2 + (bank_ctr["i"] % 6)
                        bank_ctr["i"] += 1
                        return b_

                    def blockA(l):
                        b = l % 2
                        QK32, r_QK, vst, r_vst = QK32s[b], r_QKs[b], vsts[b], r_vsts[b]
                        yield from make_hT(l, 0, 1, rstd, r_ss, h32[b], r_h32[b], hT[b], r_hT[b], (0, 1))
                        for (c0, c1) in ((0, 512), (512, 672)):
                            bank = nb()
                            for kc in range(8):
                                P.op("pe", lambda e, kc=kc, c0=c0, c1=c1, bank=bank, b=b: e.matmul(
                                    psum[bank][:, 0:c1 - c0], lhsT=hT[b][:, kc, :], rhs=win[:, kc, c0:c1],
                                    start=(kc == 0), stop=(kc == 7)),
                                    reads=[r_hT[b], r_w], writes=[ps_r[bank]], signal=(kc == 7))
                            P.op("act", lambda e, c0=c0, c1=c1, bank=bank: e.activation(
                                out=pj32[:, c0:c1], in_=psum[bank][:, 0:c1 - c0], func=ACT.Copy),
                                reads=[ps_r[bank]], writes=[r_pj])
                            yield
                        P.op("act", lambda e: e.activation(out=junkA[:, 0:384], in_=pj32[:, 0:384], func=ACT.Square,
                                                           accum_out=ssl[:, 0:1]), reads=[r_pj], writes=[r_junkA, r_ssl])
                        P.op("act", lambda e: e.activation(out=junkA[:, 0:256], in_=pj32[:, 384:640], func=ACT.Square,
                                                           accum_out=ssl[:, 1:2]), reads=[r_pj], writes=[r_junkA, r_ssl])
                        P.op("act", lambda e: e.activation(out=junkA[:, 0:32], in_=pj32[:, 640:672], func=ACT.Square,
                                                           accum_out=ssl[:, 2:3]), reads=[r_pj], writes=[r_junkA, r_ssl])
                        P.op("act", lambda e: e.activation(out=ssl[:, 0:1], in_=ssl[:, 0:1], func=ACT.Sqrt,
                                                           scale=1.0 / 384, bias=EPS), reads=[r_ssl], writes=[r_ssl])
                        P.op("act", lambda e: e.activation(out=ssl[:, 1:2], in_=ssl[:, 1:2], func=ACT.Sqrt,
                                                           scale=1.0 / 256, bias=EPS), reads=[r_ssl], writes=[r_ssl])
                        P.op("dve", lambda e: e.reciprocal(out=ssl[:, 0:2], in_=ssl[:, 0:2]), reads=[r_ssl], writes=[r_ssl])
                        yield
                        P.op("dve", lambda e: e.scalar_tensor_tensor(out=latn[:, 0:384], in0=pj32[:, 0:384],
                                                                    scalar=ssl[:, 0:1], in1=gl[:, 0:384],
                                                                    op0=ALU.mult, op1=ALU.mult),
                             reads=[r_pj, r_ssl, r_g], writes=[r_latn])
                        P.op("dve", lambda e: e.scalar_tensor_tensor(out=latn[:, 384:640], in0=pj32[:, 384:640],
                                                                    scalar=ssl[:, 1:2], in1=gl[:, 384:640],
                                                                    op0=ALU.mult, op1=ALU.mult),
                             reads=[r_pj, r_ssl, r_g], writes=[r_latn])
                        yield
                        bank = nb()
                        pT = psum[bank][:].bitcast(BF16)
                        for k5 in range(5):
                            P.op("pe", lambda e, k5=k5, pT=pT: e.transpose(
                                out=pT[:, k5 * 128:(k5 + 1) * 128], in_=latn[:, k5 * 128:(k5 + 1) * 128],
                                identity=identb[:]), reads=[r_latn, r_const], writes=[ps_r[bank]], signal=(k5 == 4))
                        P.op("act", lambda e, pT=pT: e.activation(
                            out=latT[:], in_=pT[:, 0:640].rearrange("p (k t) -> p k t", k=5), func=ACT.Copy),
                            reads=[ps_r[bank]], writes=[r_latT])
                        yield
                        qflat = QK32[:, 0].rearrange("p h d -> p (h d)")
                        for ci in range(3):
                            bank = nb()
                            for kc in range(3):
                                P.op("pe", lambda e, kc=kc, ci=ci, bank=bank: e.matmul(
                                    psum[bank][:, :], lhsT=latT[:, kc, :], rhs=wuq[:, kc, ci * 512:(ci + 1) * 512],
                                    start=(kc == 0), stop=(kc == 2)),
                                    reads=[r_latT, r_w], writes=[ps_r[bank]], signal=(kc == 2))
                            P.op("act", lambda e, ci=ci, bank=bank: e.activation(
                                out=qflat[:, ci * 512:(ci + 1) * 512], in_=psum[bank][:, :], func=ACT.Copy),
                                reads=[ps_r[bank]], writes=[r_QK])
                            yield
                        for ci in range(4):
                            bank = nb()
                            for kc in range(2):
                                P.op("pe", lambda e, kc=kc, ci=ci, bank=bank: e.matmul(
                                    psum[bank][:, :], lhsT=latT[:, 3 + kc, :], rhs=wukv[:, kc, ci * 512:(ci + 1) * 512],
                                    start=(kc == 0), stop=(kc == 1)),
                                    reads=[r_latT, r_w], writes=[ps_r[bank]], signal=(kc == 1))
                            pv = psum[bank][:, :].rearrange("p (h x) -> p h x", x=128)
                            P.op("act", lambda e, ci=ci, pv=pv: e.activation(
                                out=QK32[:, 1, ci * 4:(ci + 1) * 4, 0:64], in_=pv[:, :, 0:64], func=ACT.Copy),
                                reads=[ps_r[bank]], writes=[r_QK])
                            P.op("act", lambda e, ci=ci, pv=pv: e.activation(
                                out=vst[:, ci * 4:(ci + 1) * 4, 0:64], in_=pv[:, :, 64:128], func=ACT.Copy),
                                reads=[ps_r[bank]], writes=[r_vst])
                            yield
                        P.op("dve", lambda e: e.tensor_copy(
                            out=QK32[:, 1, :, 64:96], in_=pj32[:, 640:672].unsqueeze(1).to_broadcast([128, H, 32])),
                            reads=[r_pj], writes=[r_QK])
                        yield

                    def blockB(l):
                        b = l % 2
                        QK32, r_QK, vst, r_vst = QK32s[b], r_QKs[b], vsts[b], r_vsts[b]
                        P.op("act", lambda e: e.activation(out=SQ[:], in_=QK32[:], func=ACT.Square),
                             reads=[r_QK], writes=[r_SQ])
                        P.op("dve", lambda e: e.tensor_reduce(out=ssqk[:], in_=SQ[:], axis=AX.X, op=ALU.add),
                             reads=[r_SQ], writes=[r_ssqk])
                        yield
                        P.op("act", lambda e: e.activation(out=rsqk[:], in_=ssqk[:], func=ACT.Sqrt, scale=1.0 / 96,
                                                           bias=EPS), reads=[r_ssqk], writes=[r_ssqk])
                        P.op("dve", lambda e: e.reciprocal(out=rsqk[:], in_=rsqk[:]), reads=[r_ssqk], writes=[r_ssqk])
                        yield
                        P.op("dve", lambda e: e.tensor_tensor(
                            out=QK32[:], in0=QK32[:], in1=rsqk[:].unsqueeze(3).to_broadcast([128, 2, H, 96]),
                            op=ALU.mult), reads=[r_QK, r_ssqk], writes=[r_QK])
                        yield
                        P.op("pool", lambda e: e.tensor_tensor(
                            out=QK32[:, :, :, 64:96], in0=QK32[:, :, :, 64:96],
                            in1=gqk[:, :, 64:96].unsqueeze(2).to_broadcast([128, 2, H, 32]),
                            op=ALU.mult), reads=[r_QK, r_g], writes=[r_QK])
                        yield
                        QKv = QK32[:].rearrange("p w h d -> p (w h) d")
                        QBv = QKB[:].rearrange("p w h d -> p (w h) d")
                        x1 = QKv[:, :, 64:80]
                        x2 = QKv[:, :, 80:96]
                        sn = trig[:, 0, l, :].unsqueeze(1).to_broadcast([128, 2 * H, 16])
                        cs = trig[:, 1, l, :].unsqueeze(1).to_broadcast([128, 2 * H, 16])
                        P.op("dve", lambda e: e.tensor_tensor(
                            out=QKB[:, :, :, 0:64], in0=QK32[:, :, :, 0:64],
                            in1=gqk[:, :, 0:64].unsqueeze(2).to_broadcast([128, 2, H, 64]), op=ALU.mult),
                             reads=[r_QK, r_g], writes=[r_QKB])
                        yield
                        P.op("dve", lambda e, x1=x1, cs=cs: e.tensor_tensor(out=rt[0][:], in0=x1, in1=cs, op=ALU.mult),
                             reads=[r_QK, r_trig], writes=[r_rt])
                        P.op("pool", lambda e, x2=x2, sn=sn: e.tensor_tensor(out=rt[1][:], in0=x2, in1=sn, op=ALU.mult),
                             reads=[r_QK, r_trig], writes=[r_rt])
                        yield
                        P.op("dve", lambda e, x1=x1, sn=sn: e.tensor_tensor(out=rt[2][:], in0=x1, in1=sn, op=ALU.mult),
                             reads=[r_QK, r_trig], writes=[r_rt])
                        P.op("pool", lambda e, x2=x2, cs=cs: e.tensor_tensor(out=rt[3][:], in0=x2, in1=cs, op=ALU.mult),
                             reads=[r_QK, r_trig], writes=[r_rt])
                        yield
                        P.op("dve", lambda e: e.tensor_tensor(out=QBv[:, :, 64:80], in0=rt[0][:], in1=rt[1][:],
                                                              op=ALU.subtract), reads=[r_rt], writes=[r_QKB])
                        P.op("dve", lambda e: e.tensor_tensor(out=QBv[:, :, 80:96], in0=rt[2][:], in1=rt[3][:],
                                                              op=ALU.add), reads=[r_rt], writes=[r_QKB])
                        yield
                        sT = stT[b]
                        sTv = sT[:].rearrange("p w h t -> p (w h) t")
                        for g8 in range(4):
                            bank = nb()
                            pT = psum[bank][:].bitcast(BF16)
                            for h8 in range(8):
                                wh = g8 * 8 + h8
                                P.op("pe", lambda e, wh=wh, h8=h8, pT=pT: e.transpose(
                                    out=pT[0:96, h8 * 128:(h8 + 1) * 128], in_=QBv[:, wh, :], identity=identb[:]),
                                    reads=[r_QKB, r_const], writes=[ps_r[bank]], signal=(h8 == 7))
                            P.op("act", lambda e, g8=g8, pT=pT, sTv=sTv: e.activation(
                                out=sTv[:, g8 * 8:(g8 + 1) * 8, :],
                                in_=pT[0:96, 0:1024].rearrange("p (h t) -> p h t", h=8), func=ACT.Copy),
                                reads=[ps_r[bank]], writes=[r_stT[b]])
                            yield
                        P.dma("sp", dap(qT_d[li], l * 128, [[T, 96], [DKMAX * T, H], [1, 128]]), sT[:, 0],
                              reads=[r_stT[b]], writes=[r_qTd], merge=True)
                        for h in range(H):
                            P.dma("sp", kv_src[li][h][0:96, l * 128:(l + 1) * 128], sT[:, 1, h, :],
                                  reads=[r_stT[b]], writes=[r_kvs[h]], merge=True)
                            P.dma("sp", dap(kv_src[li][h], 96 * T + l * 65, [[NBLK * 65, 128], [1, 65]]),
                                  vst[:, h, :], reads=[r_vst], writes=[r_kvs[h]], merge=True)
                        yield

                    def run_interleaved(gens):
                        gens = list(gens)
                        while gens:
                            for g_ in list(gens):
                                try:
                                    next(g_)
                                except StopIteration:
                                    gens.remove(g_)

                    run_interleaved([blockA(0)])
                    for l in range(NBLK):
                        gl_ = [blockA(l + 1)] if l + 1 < NBLK else []
                        run_interleaved(gl_ + [blockB(l)])
                    for h in range(H):
                        P.collective(cfg.groups, kv_src[li][h].ap().opt(), kv_dst[li][h].ap().opt(),
                                     reads=[r_kvs[h]], writes=[r_kvd[h]])
                    P.barrier(skip_cc=True)
                if stop_point("coll1", P):
                    return
                o_all = sb("o_all1", [128, NBLK, D], BF16, stack=sl)
                r_o = Res("o_all1")
                attention(li, 96, 96, scale, o_all, r_o)
                if stop_point("attn1", P):
                    return
                out_proj(li, mla_wout_d, o_all, r_o)
            if stop_point("oproj1", P):
                return
            moe(li)

        if not stop_point("p0", P):
            fox_layer(0)
            if depth > 1 and not P.__dict__.get("stopped", False):
                for r_ in [r_qTd] + r_kvs + r_kvd:
                    r_.writers = {}
                    r_.readers = {}
                if not stop_point("l0", P):
                    mla_layer(1)
        for l in range(NBLK):
            P.dma("sp", y_d[l * 128:(l + 1) * 128, :], x_sb[:, l, :], f"sty{l % 4}", reads=[x_r[l]])
        P.finish()
        print("instr counts", P.ninstr, "sems", len(P.sems))
    return nc


def make_in_maps(cfg, inp):
    f32 = np.float32
    B, S, T, NBLK = cfg.B, cfg.S, cfg.T, cfg.NBLK
    depth, E = cfg.depth, cfg.E
    maps = []
    invf = np.broadcast_to((10000.0 ** (-np.arange(16, dtype=np.float32) / np.float32(16))).astype(f32)[None], (128, 16))
    kidx = np.zeros((128, 8), f32)
    for rp in range(4):
        for tp in range(2):
            kidx[:, rp * 2 + tp] = seq_block(rp, tp) * 128 + np.arange(128)
    padw = np.zeros(inp["moe_w_grp"].shape[:2] + (cfg.NRP - cfg.NR,), f32)
    padb = np.zeros(inp["moe_b_grp"].shape[:1] + (cfg.NRP - cfg.NR,), f32)
    wr = np.concatenate([inp["moe_w_grp"], inp["moe_w_rt"], padw], axis=-1).astype(f32)
    br = np.concatenate([inp["moe_b_grp"], inp["moe_b_rt"], padb], axis=-1).astype(f32)
    for b in range(B):
        for r in range(4):
            blocks = [seq_block(r, l) for l in range(NBLK)]
            xs = np.concatenate([inp["x"][b, sb_ * 128:(sb_ + 1) * 128] for sb_ in blocks], axis=0)
            ps = np.stack([inp["positions"][b, sb_ * 128:(sb_ + 1) * 128] for sb_ in blocks], axis=1)
            qidx = np.concatenate([seq_block(r, t) * 128 + np.arange(128) for t in range(2)])[None].astype(f32)
            sel = np.zeros((128, 4), f32)
            sel[:, r] = 1.0
            m = {
                "x": np.ascontiguousarray(xs, dtype=f32),
                "c": np.ascontiguousarray(inp["c"][b:b + 1], dtype=f32),
                "pos": np.ascontiguousarray(ps, dtype=np.int32),
                "qidx": qidx, "kidx": kidx, "sel": sel, "invf": invf,
                "w_ada": np.ascontiguousarray(inp["w_ada"][:, :, r * cfg.ADAW:(r + 1) * cfg.ADAW], dtype=f32),
                "b_ada": np.ascontiguousarray(inp["b_ada"][:, r * cfg.ADAW:(r + 1) * cfg.ADAW], dtype=f32),
                "mix_norm": inp["mix_norm"], "ffn_norm": inp["ffn_norm"],
                "fox_w_in": inp["fox_w_in"][0], "fox_b_f": inp["fox_b_f"][0:1],
                "fox_q_norm": inp["fox_q_norm"][0:1], "fox_k_norm": inp["fox_k_norm"][0:1],
                "fox_w_out": inp["fox_w_out"][0],
                "moe_w_r": wr, "moe_b_r": br,
                "moe_w_gate": inp["moe_w_gate"].reshape(depth * E, D, cfg.F),
                "moe_w_up": inp["moe_w_up"].reshape(depth * E, D, cfg.F),
                "moe_w_down": inp["moe_w_down"].reshape(depth * E, cfg.F, D),
            }
            if depth > 1:
                m.update({
                    "mla_w_in": inp["mla_w_in"][0], "mla_q_lat_norm": inp["mla_q_lat_norm"][0:1],
                    "mla_kv_lat_norm": inp["mla_kv_lat_norm"][0:1], "mla_w_uq": inp["mla_w_uq"][0],
                    "mla_w_ukv": inp["mla_w_ukv"][0], "mla_q_norm": inp["mla_q_norm"][0:1],
                    "mla_k_norm": inp["mla_k_norm"][0:1], "mla_w_out": inp["mla_w_out"][0],
                })
            maps.append({k: np.ascontiguousarray(v) for k, v in m.items()})
    return maps


def assemble(cfg, results):
    out = np.zeros((cfg.B, cfg.S, D), np.float32)
    for b in range(cfg.B):
        for r in range(4):
            y = results[b * 4 + r]["y"]
            for l in range(cfg.NBLK):
                sb_ = seq_block(r, l)
                out[b, sb_ * 128:(sb_ + 1) * 128] = y[l * 128:(l + 1) * 128]
    return out


def run(cfg, inp, trace=False):
    nc = build_program(cfg)
    maps = make_in_maps(cfg, inp)
    res = run_bass_kernel_spmd(nc, maps, core_ids=list(range(cfg.NCORE)), trace=trace)
    return assemble(cfg, res.results), res


def kernel(**inputs):
    inp = {k: np.asarray(v) for k, v in inputs.items()}
    cfg = Cfg(B=2, S=8192, depth=2)
    out, _ = run(cfg, inp)
    return out
```
